# Optimizing a Trainium2 kernel written in Bass

```python
import math
import jax, jax.numpy as jnp
from jax import lax
import numpy as np


D_MODEL = 1024
BATCH = 4
SEQ = 8192
DEPTH = 2

HEAD_DIM = 64
SB_HEADS = D_MODEL // 128
DIFF_HEADS = D_MODEL // 256
DIFF_VDIM = 2 * HEAD_DIM
SB_WIDTH = SB_HEADS * HEAD_DIM
DIFF_QK_WIDTH = DIFF_HEADS * 2 * HEAD_DIM
DIFF_V_WIDTH = DIFF_HEADS * DIFF_VDIM
IN_WIDTH = 3 * SB_WIDTH + 2 * DIFF_QK_WIDTH + DIFF_V_WIDTH
N_BRANCHES = 2
D_FF = 11 * D_MODEL // 4
N_EXPERTS = 8
TOP_K = 2
D_FF_EXPERT = 7 * D_MODEL // 2
BLOCK_Q = 128
MOE_BLOCK = 512
NORM_EPS = 1e-6
N_DENSE = (DEPTH + 1) // 2
N_MOE = DEPTH // 2

kernel_name = 'hybrid_stickbreak_diffattn_moe_block'


def rms_norm(x, g):
    xf = x.astype(jnp.float32)
    y = xf * lax.rsqrt(jnp.mean(xf * xf, axis=-1, keepdims=True) + NORM_EPS)
    return (y * g.astype(jnp.float32)).astype(x.dtype)


def alibi_slopes(n_heads):
    return jnp.asarray([2.0 ** (-8.0 * (h + 1) / n_heads) for h in range(n_heads)], dtype=jnp.float32)


def stick_breaking_attention(q, k, v):
    B, S, H, d = q.shape
    nb = S // BLOCK_Q
    scale = d ** -0.5
    qb = q.reshape(B, nb, BLOCK_Q, H, d).transpose(1, 0, 3, 2, 4)
    k_pos = jnp.arange(S)

    def one_block(args):
        q_blk, blk = args
        z = jnp.einsum('bhqd,bkhd->bhqk', q_blk, k).astype(jnp.float32) * scale
        q_pos = blk * BLOCK_Q + jnp.arange(BLOCK_Q)
        causal = k_pos[None, :] < q_pos[:, None]
        log_beta = jax.nn.log_sigmoid(z)
        log_keep = jnp.where(causal, jax.nn.log_sigmoid(-z), 0.0)
        log_survive = lax.cumsum(log_keep, axis=3, reverse=True) - log_keep
        w = jnp.where(causal, jnp.exp(log_beta + log_survive), 0.0)
        return jnp.einsum('bhqk,bkhd->bqhd', w.astype(v.dtype), v)

    o = lax.map(one_block, (qb, jnp.arange(nb)))
    return o.transpose(1, 0, 2, 3, 4).reshape(B, S, H * d)


def differential_attention(q, k, v, lam, lambda_init, subln_g):
    B, S, H, _, d = q.shape
    nb = S // BLOCK_Q
    scale = d ** -0.5
    slopes = alibi_slopes(H)
    qb = q.reshape(B, nb, BLOCK_Q, H, 2, d).transpose(1, 0, 3, 4, 2, 5)
    k_pos = jnp.arange(S)

    def one_block(args):
        q_blk, blk = args
        s = jnp.einsum('bhcqd,bkhcd->bhcqk', q_blk, k).astype(jnp.float32) * scale
        q_pos = blk * BLOCK_Q + jnp.arange(BLOCK_Q)
        dist = q_pos[:, None] - k_pos[None, :]
        bias = -slopes[:, None, None] * dist.astype(jnp.float32)
        s = jnp.where(dist >= 0, s + bias[None, :, None], -jnp.inf)
        p = jax.nn.softmax(s, axis=-1)
        attn = p[:, :, 0] - lam * p[:, :, 1]
        return jnp.einsum('bhqk,bkhe->bqhe', attn.astype(v.dtype), v)

    o = lax.map(one_block, (qb, jnp.arange(nb)))
    o = o.transpose(1, 0, 2, 3, 4).reshape(B, S, H, 2 * d)
    o = rms_norm(o, subln_g) * (1.0 - lambda_init)
    return o.reshape(B, S, H * 2 * d)


def swiglu(h, w_gate_up, w_down):
    g, u = jnp.split(h @ w_gate_up, 2, axis=-1)
    return (jax.nn.silu(g) * u) @ w_down


def moe_swiglu(h, w_router, w1, w3, w2):
    B, S, D = h.shape
    n_tok = B * S
    n_assign = n_tok * TOP_K
    n_slots = -(-n_assign // MOE_BLOCK) * MOE_BLOCK + N_EXPERTS * MOE_BLOCK
    n_blocks = n_slots // MOE_BLOCK
    t = h.reshape(n_tok, D)
    logits = (t @ w_router).astype(jnp.float32)
    top_logit, top_idx = lax.top_k(logits, TOP_K)
    top_w = jax.nn.softmax(top_logit, axis=-1)
    flat_e = top_idx.reshape(-1)
    flat_tok = jnp.repeat(jnp.arange(n_tok, dtype=jnp.int32), TOP_K)
    flat_w = top_w.reshape(-1)
    order = jnp.argsort(flat_e)
    e_sorted = flat_e[order]
    counts = jnp.bincount(flat_e, length=N_EXPERTS)
    padded = (counts + MOE_BLOCK - 1) // MOE_BLOCK * MOE_BLOCK
    start = jnp.cumsum(counts) - counts
    padded_end = jnp.cumsum(padded)
    padded_start = padded_end - padded
    slot = padded_start[e_sorted] + jnp.arange(n_assign) - start[e_sorted]
    slot_tok = jnp.zeros((n_slots,), jnp.int32).at[slot].set(flat_tok[order])
    slot_w = jnp.zeros((n_slots,), jnp.float32).at[slot].set(flat_w[order])
    block_e = jnp.minimum(
        jnp.searchsorted(padded_end, jnp.arange(n_blocks) * MOE_BLOCK, side='right'),
        N_EXPERTS - 1)
    xs = t[slot_tok].reshape(n_blocks, MOE_BLOCK, D)

    def expert_block(args):
        xb, e = args
        hid = jax.nn.silu(xb @ w1[e]) * (xb @ w3[e])
        return hid @ w2[e]

    ys = lax.map(expert_block, (xs, block_e)).reshape(n_slots, D)
    ys = ys * slot_w[:, None].astype(ys.dtype)
    out = jax.ops.segment_sum(ys, slot_tok, num_segments=n_tok)
    return out.reshape(B, S, D)


def _normal(k, shape, scale):
    return jax.random.normal(k, shape, jnp.float32) * scale


def setup_inputs(seed: int = 0) -> dict:
    key = jax.random.key(seed)
    ks = jax.random.split(key, 20)
    D = D_MODEL
    return {
        'x': _normal(ks[0], (BATCH, SEQ, D), 1.0),
        'norm_mix_g': 1.0 + _normal(ks[1], (DEPTH, D), 0.01),
        'w_in': _normal(ks[2], (DEPTH, D, IN_WIDTH), D ** -0.5),
        'diff_q_norm_g': 1.0 + _normal(ks[3], (DEPTH, HEAD_DIM), 0.01),
        'diff_k_norm_g': 1.0 + _normal(ks[4], (DEPTH, HEAD_DIM), 0.01),
        'diff_lambda': _normal(ks[5], (DEPTH, 4, HEAD_DIM), 0.1),
        'diff_subln_g': 1.0 + _normal(ks[6], (DEPTH, DIFF_VDIM), 0.01),
        'w_gate': _normal(ks[7], (DEPTH, D, N_BRANCHES * D), D ** -0.5),
        'b_gate': _normal(ks[8], (DEPTH, N_BRANCHES * D), 0.02),
        'w_branch_sb': _normal(ks[9], (DEPTH, SB_WIDTH, D), SB_WIDTH ** -0.5),
        'w_branch_diff': _normal(ks[10], (DEPTH, DIFF_V_WIDTH, D), DIFF_V_WIDTH ** -0.5),
        'w_out': _normal(ks[11], (DEPTH, D, D), D ** -0.5),
        'norm_ffn_g': 1.0 + _normal(ks[12], (DEPTH, D), 0.01),
        'ffn_w_gate_up': _normal(ks[13], (N_DENSE, D, 2 * D_FF), D ** -0.5),
        'ffn_w_down': _normal(ks[14], (N_DENSE, D_FF, D), D_FF ** -0.5),
        'moe_w_router': _normal(ks[15], (N_MOE, D, N_EXPERTS), D ** -0.5),
        'moe_w1': _normal(ks[16], (N_MOE, N_EXPERTS, D, D_FF_EXPERT), D ** -0.5),
        'moe_w3': _normal(ks[17], (N_MOE, N_EXPERTS, D, D_FF_EXPERT), D ** -0.5),
        'moe_w2': _normal(ks[18], (N_MOE, N_EXPERTS, D_FF_EXPERT, D), D_FF_EXPERT ** -0.5),
    }


def reference(x, norm_mix_g, w_in, diff_q_norm_g, diff_k_norm_g, diff_lambda, diff_subln_g,
              w_gate, b_gate, w_branch_sb, w_branch_diff, w_out, norm_ffn_g,
              ffn_w_gate_up, ffn_w_down, moe_w_router, moe_w1, moe_w3, moe_w2):
    B, S, D = x.shape
    split_at = [int(c) for c in np.cumsum([SB_WIDTH, SB_WIDTH, SB_WIDTH, DIFF_QK_WIDTH, DIFF_QK_WIDTH])]
    for i in range(DEPTH):
        h = rms_norm(x, norm_mix_g[i])
        proj = h @ w_in[i]
        sb_q, sb_k, sb_v, df_q, df_k, df_v = jnp.split(proj, split_at, axis=-1)
        y_sb = stick_breaking_attention(
            sb_q.reshape(B, S, SB_HEADS, HEAD_DIM),
            sb_k.reshape(B, S, SB_HEADS, HEAD_DIM),
            sb_v.reshape(B, S, SB_HEADS, HEAD_DIM))
        df_q = rms_norm(df_q.reshape(B, S, DIFF_HEADS, 2, HEAD_DIM), diff_q_norm_g[i])
        df_k = rms_norm(df_k.reshape(B, S, DIFF_HEADS, 2, HEAD_DIM), diff_k_norm_g[i])
        lambda_init = 0.8 - 0.6 * math.exp(-0.3 * i)
        lq1, lk1, lq2, lk2 = diff_lambda[i].astype(jnp.float32)
        lam = jnp.exp(jnp.sum(lq1 * lk1)) - jnp.exp(jnp.sum(lq2 * lk2)) + lambda_init
        y_df = differential_attention(
            df_q, df_k, df_v.reshape(B, S, DIFF_HEADS, DIFF_VDIM),
            lam, lambda_init, diff_subln_g[i])
        gates = jax.nn.sigmoid(h @ w_gate[i] + b_gate[i]).reshape(B, S, N_BRANCHES, D)
        merged = gates[:, :, 0] * (y_sb @ w_branch_sb[i]) + gates[:, :, 1] * (y_df @ w_branch_diff[i])
        x = x + merged @ w_out[i]
        h = rms_norm(x, norm_ffn_g[i])
        j = i // 2
        if i % 2 == 0:
            x = x + swiglu(h, ffn_w_gate_up[j], ffn_w_down[j])
        else:
            x = x + moe_swiglu(h, moe_w_router[j], moe_w1[j], moe_w3[j], moe_w2[j])
    return x
```

```python
import math
from contextlib import ExitStack

import numpy as np
import concourse.bass as bass
import concourse.mybir as mybir
from concourse.bass_utils import run_bass_kernel_spmd

F32 = mybir.dt.float32
BF16 = mybir.dt.bfloat16
AF = mybir.ActivationFunctionType
ALU = mybir.AluOpType
AX = mybir.AxisListType

D = 1024
EPS = 1e-6
GSLOT = [[0, 3], [1, 2]]
N_EXP = 8
DFF = 2816
DFE = 3584
NEG_BIG = -1.0e30


class Sem:
    def __init__(self, h, is_dma):
        self.h = h
        self.total = 0
        self.is_dma = is_dma


class T:
    def __init__(self, t):
        self.t = t
        self.lw = None
        self.rd = {}
        self.dsem = None

    def __getitem__(self, idx):
        return self.t[idx]


class Eng:
    def __init__(self, name, eng, sem):
        self.name = name
        self.eng = eng
        self.sem = sem
        self.seen = {}


class Cx:
    def __init__(self, nc, stack):
        self.nc = nc
        self.stack = stack
        self.E = {}
        self.allsems = []
        for name, eng in (("pe", nc.tensor), ("act", nc.scalar), ("dve", nc.vector),
                          ("pool", nc.gpsimd), ("sp", nc.sync)):
            s = Sem(stack.enter_context(nc.semaphore("s_" + name)), False)
            self.allsems.append(s)
            self.E[name] = Eng(name, eng, s)
        self.free_dsems = {}
        self.live_dsems = []
        self.n_dsem = 0
        self.rr = 0

    def sb(self, ph, shape, dt, name=None):
        self.rr += 1
        return T(ph.enter_context(self.nc.sbuf_tensor(f"sb{self.rr}_{name or 't'}", list(shape), dt)))

    def ps(self, ph, shape, dt=F32, name=None):
        self.rr += 1
        return T(ph.enter_context(self.nc.psum_tensor(f"ps{self.rr}_{name or 'p'}", list(shape), dt)))

    def dsem_for(self, tb, kind="hw"):
        if tb.dsem is None:
            tb.dsem = {}
        if kind not in tb.dsem:
            fl = self.free_dsems.setdefault(kind, [])
            if fl:
                s = fl.pop()
            else:
                self.n_dsem += 1
                s = Sem(self.stack.enter_context(self.nc.semaphore(f"d{self.n_dsem}")), True)
                self.allsems.append(s)
            self.live_dsems.append((kind, s))
            tb.dsem[kind] = s
        return tb.dsem[kind]

    def release_phase(self):
        for kind, s_ in self.live_dsems:
            self.free_dsems.setdefault(kind, []).append(s_)
        self.live_dsems = []

    def _sync(self, E, reads, writes):
        need = {}

        def add(tk):
            if tk is None:
                return
            s, v = tk
            if s.is_dma:
                v = s.total
            if s is E.sem and E.name in ("pe", "sp"):
                return
            cur = need.get(id(s))
            if cur is None or cur[1] < v:
                need[id(s)] = (s, v)

        for b in reads:
            add(b.lw)
        for b in writes:
            add(b.lw)
            for tk in b.rd.values():
                add(tk)
        for s, v in need.values():
            if E.seen.get(id(s), 0) >= v:
                continue
            E.eng.wait_ge(s.h, v)
            E.seen[id(s)] = v

    def _reg(self, tk, reads, writes):
        s = tk[0]
        for b in reads:
            b.rd[id(s)] = tk
        for b in writes:
            b.lw = tk
            b.rd = {}

    def op(self, en, fn, reads=(), writes=()):
        E = self.E[en]
        self._sync(E, reads, writes)
        ins = fn(E.eng)
        E.sem.total += 1
        ins.then_inc(E.sem.h, 1)
        self._reg((E.sem, E.sem.total), reads, writes)

    def dma(self, qn, out, in_, reads=(), writes=(), semb=None, **kw):
        Q = self.E[qn]
        self._sync(Q, reads, writes)
        ds = self.dsem_for(semb, "sw" if qn == "pool" else "hw")
        ins = Q.eng.dma_start(out=out, in_=in_, **kw)
        ds.total += 16
        ins.then_inc(ds.h, 16)
        self._reg((ds, ds.total), reads, writes)

    def barrier(self, release=True):
        self._barrier()
        if release:
            self.release_phase()

    def _barrier(self):
        for E in self.E.values():
            for s in self.allsems:
                if s.total == 0:
                    continue
                if E.seen.get(id(s), 0) >= s.total:
                    continue
                E.eng.wait_ge(s.h, s.total)
                E.seen[id(s)] = s.total


def act(cx, out, in_, func, reads, writes, **kw):
    cx.op("act", lambda e: e.activation(out=out, in_=in_, func=func, **kw), reads, writes)


def mm(cx, out, lhsT, rhs, start, stop, reads, writes, **kw):
    cx.op("pe", lambda e: e.matmul(out, lhsT=lhsT, rhs=rhs, start=start, stop=stop, **kw), reads, writes)


def core_gi(p):
    return [GSLOT[p][0], GSLOT[p][1], 4 + GSLOT[p][0], 4 + GSLOT[p][1]]


def host_consts(p, U):
    gi = core_gi(p)
    s = np.arange(128)[:, None]
    t = np.arange(128)[None, :]
    maskS = np.zeros((128, 8, 512), np.float32)
    maskD = np.zeros((128, 8, 512), np.float32)
    for r in range(8):
        for i in range(4):
            sg = r * 128 + s
            tg = gi[i] * 128 + t
            maskS[:, r, i * 128:(i + 1) * 128] = (sg < tg)
            maskD[:, r, i * 128:(i + 1) * 128] = (sg <= tg)
    j = np.arange(128)[:, None]
    negU = -(j >= np.arange(128)[None, :]).astype(np.float32)
    ident = np.eye(128, dtype=np.float32)
    bd = np.zeros((128, 128), np.float32)
    bd[:64, :64] = 1.0
    bd[64:, 64:] = 1.0
    S = U * 128
    nslot = S // 256
    kx = np.zeros((4, 2, S), np.float32)
    qx = np.zeros((4, 2, S // 2), np.float32)
    sidx = np.arange(S)
    tblk = np.repeat(np.array([4 * (j // 2) + GSLOT[p][j % 2] for j in range(nslot)]), 128)
    tloc = np.tile(np.arange(128), nslot)
    for h in range(4):
        m = 2.0 ** (-8.0 * (h + 1) / 4)
        kx[h, 0] = m * 128.0 * (sidx // 128)
        kx[h, 1] = m * (sidx % 128)
        qx[h, 0] = -m * 128.0 * tblk
        qx[h, 1] = -m * tloc
    ltri = (np.arange(128)[:, None] < np.arange(128)[None, :]).astype(np.float32)
    s128p = (np.arange(14)[None, :] * 128.0 + np.arange(128)[:, None]).astype(np.float32)
    return dict(maskS=maskS, maskD=maskD, negU=negU, ident=ident, bd=bd, kx=kx, qx=qx, ltri=ltri, s128p=s128p)


class Prog:
    def __init__(self, S, layers, debug=False, ncores=8):
        self.ncores = ncores
        self.S = S
        self.S2 = S // 2
        self.NQ = S // 1024
        self.NB = S // 128
        self.NSLOT = S // 256
        self.U = self.NB
        self.layers = layers
        self.debug = debug
        self.nc = bass.Bass("TRN2", target_bir_lowering=False)
        self.dr = {}

    def din(self, name, shape, dt=F32):
        self.dr[name] = self.nc.dram_tensor(name, list(shape), dt, kind="ExternalInput").ap()
        return self.dr[name]

    def dout(self, name, shape, dt=F32):
        self.dr[name] = self.nc.dram_tensor(name, list(shape), dt, kind="ExternalOutput").ap()
        return self.dr[name]

    def dscr(self, name, shape, dt):
        self.dr[name] = self.nc.dram_tensor(name, list(shape), dt).ap()
        return self.dr[name]

    def build(self):
        S, S2 = self.S, self.S2
        d = self.dr
        self.din("xfull", [S, D])
        self.din("xown", [S2, D])
        for nm, shp in (("maskS", [128, 8, 512]), ("maskD", [128, 8, 512]), ("negU", [128, 128]),
                        ("ident", [128, 128]), ("bd", [128, 128]), ("kx", [4, 2, S]),
                        ("qx", [4, 2, S2])):
            self.din(nm, shp)
        for L in self.layers:
            sfx = f"_{L}"
            self.din("gmix" + sfx, [128, D])
            self.din("w_in" + sfx, [D, 3072])
            self.din("gq" + sfx, [128, 1])
            self.din("gk" + sfx, [128, 1])
            self.din("gqk_row" + sfx, [1, 128])
            self.din("lam_row" + sfx, [1, 256])
            self.din("gsub" + sfx, [128, 128])
            self.din("w_gate" + sfx, [D, 2048])
            self.din("b_gate" + sfx, [1, 2048])
            self.din("w_bsb" + sfx, [512, D])
            self.din("w_bdf" + sfx, [512, D])
            self.din("w_out" + sfx, [D, D])
            self.din("gffn" + sfx, [128, D])
            if L % 2 == 0:
                self.din("w_gu" + sfx, [D, 2 * DFF])
                self.din("w_dn" + sfx, [DFF, D])
            else:
                self.din("w_r" + sfx, [D, N_EXP])
                self.din("w1r" + sfx, [N_EXP * 14 * 128, 2048])
                self.din("w3r" + sfx, [N_EXP * 14 * 128, 2048])
                self.din("w2r" + sfx, [N_EXP * 14 * 128, 2048])
                self.has_moe = True
        self.dout("xout", [S2, D])
        if self.debug:
            self.dout("dbg_ysb", [S2, 512])
            self.dout("dbg_ydf", [S2, 512])
            self.dout("dbg_x1", [S2, D])
        self.dscr("KT", [D, S], BF16)
        self.dscr("QT", [D, S2], BF16)
        self.dscr("V", [S, D], BF16)
        self.dscr("X1", [S2, D], F32)
        self.dscr("H2T", [D, S2], BF16)
        if getattr(self, "has_moe", False):
            self.NBLK = S2 // 256 + 8
            self.din("ltri", [128, 128])
            self.din("s128p", [128, 14])
            self.dscr("H2", [S2, D], BF16)
            self.dscr("XS", [self.NBLK * 512, D], BF16)
            self.dscr("YS", [self.NBLK * 512, D], F32)
        if len(self.layers) > 1:
            self.dscr("XOWN2", [S2, D], F32)
            self.dscr("XG2", [2, S2, D], F32)
            self.dscr("XF2", [2, S2, D], F32)
            self.din("pmask", [128, 2])

        nc = self.nc
        with ExitStack() as stack:
            cx = Cx(nc, stack)
            self.cx = cx
            self.tKT, self.tQT, self.tV, self.tX1, self.tH2T = T(None), T(None), T(None), T(None), T(None)
            self.tXO = T(None)
            nl = len(self.layers)
            self.tXF = T(None)
            for li, L in enumerate(self.layers):
                last = (li == nl - 1)
                self.first_layer = (li == 0)
                if not last:
                    self.ccs = Sem(stack.enter_context(nc.semaphore(f"cc{li}")), False)
                if li == 0:
                    kv_row = lambda g: d["xfull"][g * 128:(g + 1) * 128, :]
                    xown = d["xown"]
                else:
                    def kv_row(g):
                        pp = 0 if (g % 4) in (0, 3) else 1
                        j = 2 * (g // 4) + (0 if (g % 4) in (0, 1) else 1)
                        return d["XF2"][pp, j * 128:(j + 1) * 128, :]
                    xown = d["XOWN2"]
                self.cur_xown = xown
                with ExitStack() as lst:
                    self.lst = lst
                    if L % 2 == 1:
                        self.rw = cx.sb(lst, [128, self.NSLOT, N_EXP], F32, "rw")
                        self.selA = cx.sb(lst, [128, self.NSLOT, N_EXP], F32, "selA")
                        self.oh1A = cx.sb(lst, [128, self.NSLOT, N_EXP], F32, "oh1A")
                    self.phase_proj(L, kv_row, xown)
                    cx.barrier()
                    self.phase_attn(L)
                    cx.barrier()
                    if L % 2 == 1:
                        assert last
                        self.phase_moe_sparse(L)
                    else:
                        self.phase_ffn(L, last)
                    cx.barrier()
        return nc

    def nt_a(self, cx, row_ap, xt, ss, rt, rstd, xn, gbc, extra_reads=()):
        cx.dma("sp", xt[:, :], row_ap, reads=list(extra_reads), writes=[xt], semb=xt)
        act(cx, xn[:, :], xt[:, :], AF.Square, [xt], [xn, ss], accum_out=ss[:, :])
        act(cx, rt[:, :], ss[:, :], AF.Sqrt, [ss], [rt], scale=1.0 / D, bias=EPS)
        cx.op("dve", lambda e: e.reciprocal(out=rstd[:, :], in_=rt[:, :]), [rt], [rstd])
        cx.op("dve", lambda e: e.scalar_tensor_tensor(out=xn[:, :], in0=xt[:, :], scalar=rstd[:, :],
                                                      in1=gbc[:, :], op0=ALU.mult, op1=ALU.mult),
              [xt, rstd, gbc], [xn])

    def nt_b(self, cx, xn, pT, ident, dst, dcol, eng="act"):
        for c in range(8):
            cx.op("pe", lambda e: e.transpose(pT[:, c * 128:(c + 1) * 128], xn[:, c * 128:(c + 1) * 128],
                                              ident[:, :]), [xn, ident], [pT])
        if eng == "act":
            cx.op("act", lambda e: e.copy(out=dst[:, :, dcol:dcol + 128],
                                          in_=pT[:, :].rearrange("p (c t) -> p c t", c=8)), [pT], [dst])
        else:
            cx.op("dve", lambda e: e.tensor_copy(out=dst[:, :, dcol:dcol + 128],
                                                 in_=pT[:, :].rearrange("p (c t) -> p c t", c=8)), [pT], [dst])

    def norm_transpose(self, cx, row_ap, xt, ss, rt, rstd, xn, pT, gbc, ident, dst, dcol):
        self.nt_a(cx, row_ap, xt, ss, rt, rstd, xn, gbc)
        self.nt_b(cx, xn, pT, ident, dst, dcol)

    def phase_proj(self, L, kv_row, xown):
        cx, nc, d = self.cx, self.nc, self.dr
        sfx = f"_{L}"
        S, S2 = self.S, self.S2
        with ExitStack() as ph:
            wt = cx.sb(ph, [128, 8, 3072], BF16, "wt")
            wv = d["w_in" + sfx].rearrange("(c p) n -> p c n", p=128)
            wtg = [T(wt.t) for _ in range(6)]
            order = [1, 4, 2, 5, 0, 3] if self.first_layer else [0, 3, 1, 4, 2, 5]
            for g_ in order:
                for j in (2 * g_, 2 * g_ + 1):
                    cx.dma("pool", wt[:, :, j * 256:(j + 1) * 256], wv[:, :, j * 256:(j + 1) * 256],
                           writes=[wtg[g_]], semb=wtg[g_])
            gbc = cx.sb(ph, [128, D], F32, "gbc")
            cx.dma("sp", gbc[:, :], d["gmix" + sfx][:, :], writes=[gbc], semb=gbc)
            ident = cx.sb(ph, [128, 128], BF16, "ident")
            cx.dma("pool", ident[:, :], d["ident"][:, :], writes=[ident], semb=ident)
            bd = cx.sb(ph, [128, 128], BF16, "bd")
            cx.dma("pool", bd[:, :], d["bd"][:, :], writes=[bd], semb=bd)
            gq = cx.sb(ph, [128, 1], F32, "gq")
            gk = cx.sb(ph, [128, 1], F32, "gk")
            cx.dma("sp", gq[:, :], d["gq" + sfx][:, :], writes=[gq], semb=gq)
            cx.dma("sp", gk[:, :], d["gk" + sfx][:, :], writes=[gk], semb=gk)
            gq8 = cx.sb(ph, [128, 1], F32, "gq8")
            cx.op("dve", lambda e: e.tensor_scalar(out=gq8[:, :], in0=gq[:, :], scalar1=0.125, scalar2=None,
                                                   op0=ALU.mult), [gq], [gq8])
            xts = [cx.sb(ph, [128, D], F32, f"xt{i}") for i in range(4)]
            xns = [cx.sb(ph, [128, D], BF16, f"xn{i}") for i in range(4)]
            sss = [cx.sb(ph, [128, 1], F32, f"ss{i}") for i in range(4)]
            rts = [cx.sb(ph, [128, 1], F32, f"rt{i}") for i in range(4)]
            rss = [cx.sb(ph, [128, 1], F32, f"rs{i}") for i in range(4)]
            pTs = [cx.ps(ph, [128, D], BF16, f"pT{i}") for i in range(2)]
            hTs = [cx.sb(ph, [128, 8, 512], BF16, f"hT{i}") for i in range(2)]
            pms = [cx.ps(ph, [128, 512], F32, f"pm{i}") for i in range(4)]
            stg = [cx.sb(ph, [128, 512], BF16, f"stg{i}") for i in range(4)]
            sqs = [cx.sb(ph, [128, 512], BF16, f"sq{i}") for i in range(2)]
            rinv = [cx.sb(ph, [128, 512], F32, f"rinv{i}") for i in range(2)]
            cnt = {"x": 0, "pm": 0, "stg": 0, "sq": 0}

            def fm_proj(hT, col0, fc, kind, dst_t, dst_ap_fn):
                if True:
                    pm = pms[cnt["pm"] % 4]
                    cnt["pm"] += 1
                    c0 = col0 + fc * 128
                    for c in range(8):
                        mm(cx, pm[:, :], wt[:, c, c0:c0 + 128], hT[:, c, :], c == 0, c == 7, [wtg[c0 // 512], hT], [pm])
                    st = stg[cnt["stg"] % 4]
                    cnt["stg"] += 1
                    if kind == "sbk":
                        cx.op("dve", lambda e: e.tensor_copy(out=st[:, :], in_=pm[:, :]), [pm], [st])
                    elif kind == "sbq":
                        act(cx, st[:, :], pm[:, :], AF.Copy, [pm], [st], scale=0.125)
                    else:
                        sq = sqs[cnt["sq"] % 2]
                        rv = rinv[cnt["sq"] % 2]
                        cnt["sq"] += 1
                        pm2 = pms[cnt["pm"] % 4]
                        cnt["pm"] += 1
                        act(cx, sq[:, :], pm[:, :], AF.Square, [pm], [sq])
                        mm(cx, pm2[:, :], bd[:, :], sq[:, :], True, True, [bd, sq], [pm2])
                        act(cx, rv[:, :], pm2[:, :], AF.Sqrt, [pm2], [rv], scale=1.0 / 64, bias=EPS)
                        cx.op("dve", lambda e: e.reciprocal(out=rv[:, :], in_=rv[:, :]), [rv], [rv])
                        gcol = gq8 if kind == "dfq" else gk
                        cx.op("dve", lambda e: e.scalar_tensor_tensor(
                            out=st[:, :], in0=pm[:, :], scalar=gcol[:, :], in1=rv[:, :],
                            op0=ALU.mult, op1=ALU.mult), [pm, gcol, rv], [st])
                    cx.dma("sp", dst_ap_fn(fc), st[:, :], reads=[st], writes=[], semb=st)

            def tm_proj(hT, u, col0, dst_t, dst_ap):
                pm = pms[cnt["pm"] % 4]
                cnt["pm"] += 1
                for c in range(8):
                    mm(cx, pm[:, :], hT[:, c, u * 128:(u + 1) * 128], wt[:, c, col0:col0 + 512],
                       c == 0, c == 7, [wtg[col0 // 512], hT], [pm])
                st = stg[cnt["stg"] % 4]
                cnt["stg"] += 1
                cx.op("dve", lambda e: e.tensor_copy(out=st[:, :], in_=pm[:, :]), [pm], [st])
                cx.dma("sp", dst_ap, st[:, :], reads=[st], writes=[], semb=st)

            sts_kv = [("kv", kv_row, i) for i in range(S // 512)]
            sts_q = [("q", (lambda g: xown[g * 128:(g + 1) * 128, :]), i) for i in range(S2 // 512)]
            sts = (sts_kv + sts_q) if self.first_layer else (sts_q + sts_kv)

            def chunks_of(n):
                kind, src, st_i = sts[n]
                hT = hTs[n % 2]
                t0 = st_i * 512
                out = []
                if kind == "kv":
                    for fc in range(4):
                        out.append(lambda fc=fc: fm_proj(hT, 512, fc, "sbk", None,
                                                         lambda f: d["KT"][f * 128:(f + 1) * 128, t0:t0 + 512]))
                    for fc in range(4):
                        out.append(lambda fc=fc: fm_proj(hT, 2048, fc, "dfk", None,
                                                         lambda f: d["KT"][512 + f * 128:512 + (f + 1) * 128, t0:t0 + 512]))
                    for u in range(4):
                        r0 = t0 + u * 128
                        out.append(lambda u=u, r0=r0: tm_proj(hT, u, 1024, None, d["V"][r0:r0 + 128, 0:512]))
                        out.append(lambda u=u, r0=r0: tm_proj(hT, u, 2560, None, d["V"][r0:r0 + 128, 512:1024]))
                else:
                    for fc in range(4):
                        out.append(lambda fc=fc: fm_proj(hT, 0, fc, "sbq", None,
                                                         lambda f: d["QT"][f * 128:(f + 1) * 128, t0:t0 + 512]))
                    for fc in range(4):
                        out.append(lambda fc=fc: fm_proj(hT, 1536, fc, "dfq", None,
                                                         lambda f: d["QT"][512 + f * 128:512 + (f + 1) * 128, t0:t0 + 512]))
                return out

            def a_part(n, u):
                kind, src, st_i = sts[n]
                xr = [self.tXF] if (kind == "kv" and not self.first_layer) else []
                self.nt_a(cx, src(st_i * 4 + u), xts[u], sss[u], rts[u], rss[u], xns[u], gbc, extra_reads=xr)

            def b_part(n, u):
                self.nt_b(cx, xns[u], pTs[u % 2], ident, hTs[n % 2], u * 128)

            for u in range(4):
                a_part(0, u)
            for u in range(4):
                b_part(0, u)
            for n in range(len(sts)):
                ch = chunks_of(n)
                per = len(ch) // 4
                nxt = n + 1 < len(sts)
                for i, fn in enumerate(ch):
                    if nxt and i < 4:
                        a_part(n + 1, i)
                    fn()
                    if nxt and (i + 1) % per == 0 and False:
                        pass
                if nxt:
                    for u in range(4):
                        b_part(n + 1, u)
            cx.barrier()

    def tiles(self):
        out = []
        for q in range(self.NQ):
            nkb = 8 * q + 8
            for u in range(nkb):
                kb = nkb - 1 - u
                r = kb - 8 * q
                i0 = (r // 2) if r >= 0 else 0
                out.append(dict(q=q, u=u, kb=kb, r=r, i0=i0, t0=128 * i0, first=(u == 0), last=(u == nkb - 1)))
        return out

    def phase_attn(self, L):
        cx, nc, d = self.cx, self.nc, self.dr
        sfx = f"_{L}"
        S, S2, NB = self.S, self.S2, self.NB
        lam_init = 0.8 - 0.6 * math.exp(-0.3 * L)
        with ExitStack() as ph0:
            ysb = cx.sb(ph0, [128, self.NSLOT, 512], BF16, "ysb")
            ydf = cx.sb(ph0, [128, self.NSLOT, 512], BF16, "ydf")
            self.ysb, self.ydf = ysb, ydf
            tl = self.tiles()
            with ExitStack() as ph:
                maskS = cx.sb(ph, [128, 8, 512], BF16, "maskS")
                cx.dma("pool", maskS[:, :, :], d["maskS"][:, :, :], writes=[maskS], semb=maskS)
                cx.op("dve", lambda e: e.tensor_scalar(out=maskS[:, :, :], in0=maskS[:, :, :], scalar1=-1.0,
                                                       scalar2=30000.0, op0=ALU.add, op1=ALU.mult), [maskS], [maskS])
                identA = cx.sb(ph, [128, 128], BF16, "identA")
                cx.dma("pool", identA[:, :], d["ident"][:, :], writes=[identA], semb=identA)
                negU = cx.sb(ph, [128, 128], BF16, "negU")
                cx.dma("pool", negU[:, :], d["negU"][:, :], writes=[negU], semb=negU)
                ones = cx.sb(ph, [128, 1], BF16, "ones")
                cx.op("dve", lambda e: e.memset(ones[:, :], 1.0), [], [ones])
                kts = [cx.sb(ph, [128, S], BF16, f"kt{i}") for i in range(2)]
                qts = [cx.sb(ph, [128, S2], BF16, f"qt{i}") for i in range(2)]
                vvs = [cx.sb(ph, [128, NB, 128], BF16, f"vv{i}") for i in range(2)]
                psA = [cx.ps(ph, [128, 2, 512], F32, f"psA{i}") for i in range(3)]
                psO = cx.ps(ph, [128, 8, 64], F32, "psO")
                psC = cx.ps(ph, [128, 512], F32, "psC")
                eb = [cx.sb(ph, [128, 2, 512], F32, f"eb{i}") for i in range(2)]
                spb = [cx.sb(ph, [128, 2, 512], BF16, f"spb{i}") for i in range(2)]
                wtb = [cx.sb(ph, [128, 2, 512], BF16, f"wtb{i}") for i in range(2)]
                fb = [cx.sb(ph, [128, 2, 4], F32, f"fb{i}") for i in range(2)]
                oacc = [cx.sb(ph, [128, 64], F32, f"oacc{i}") for i in range(4)]
                cbuf = [cx.sb(ph, [128, 2, 4], F32, f"carry{i}") for i in range(3)]
                pairs = [(tl[2 * m], tl[2 * m + 1]) for m in range(len(tl) // 2)]
                n = len(pairs)

                def load_hp(hp_):
                    kt_, qt_, vv_ = kts[hp_ % 2], qts[hp_ % 2], vvs[hp_ % 2]
                    cx.dma("sp", kt_[:, :], d["KT"][hp_ * 128:(hp_ + 1) * 128, :], reads=[], writes=[kt_], semb=kt_)
                    cx.dma("sp", qt_[:, :], d["QT"][hp_ * 128:(hp_ + 1) * 128, :], reads=[], writes=[qt_], semb=qt_)
                    cx.dma("sp", vv_[:, :, :],
                           d["V"][:, hp_ * 128:(hp_ + 1) * 128].rearrange("(b p) c -> p b c", p=128),
                           reads=[], writes=[vv_], semb=vv_)

                load_hp(0)
                for hp in range(4):
                    kt, qt, vv = kts[hp % 2], qts[hp % 2], vvs[hp % 2]
                    if hp + 1 < 4:
                        load_hp(hp + 1)
                    for hh in range(2):
                        h = 2 * hp + hh
                        rows = slice(hh * 64, hh * 64 + 64)

                        def s1(m):
                            A = psA[m % 3]
                            for x, t in enumerate(pairs[m]):
                                t0, kb = t["t0"], t["kb"]
                                mm(cx, A[:, x, t0:512], kt[rows, kb * 128:(kb + 1) * 128],
                                   qt[rows, t["q"] * 512 + t0:(t["q"] + 1) * 512], True, False, [kt, qt], [A],
                                   skip_group_check=True)
                                if t["r"] >= 0:
                                    mm(cx, A[:, x, t0:512], identA[:, :], maskS[:, t["r"], t0:512], False, False,
                                       [identA, maskS], [A], skip_group_check=True)

                        def sE(m):
                            t0 = pairs[m][0]["t0"]
                            A, e_ = psA[m % 3], eb[m % 2]
                            act(cx, e_[:, :, t0:512], A[:, :, t0:512], AF.Exp, [A], [e_])

                        def sSP(m):
                            t0 = pairs[m][0]["t0"]
                            e_, sp = eb[m % 2], spb[m % 2]
                            act(cx, sp[:, :, t0:512], e_[:, :, t0:512], AF.Ln, [e_], [sp], bias=1.0)

                        def sBC(m):
                            B, sp, C = psA[m % 3], spb[m % 2], psC
                            t0, i0 = pairs[m][0]["t0"], pairs[m][0]["i0"]
                            for x, t in enumerate(pairs[m]):
                                mm(cx, B[:, x, t0:512], negU[:, :], sp[:, x, t0:512], False, True, [negU, sp], [B],
                                   skip_group_check=True)
                            cc, cn = cbuf[m % 3], cbuf[(m + 1) % 3]
                            if pairs[m][0]["first"]:
                                cx.op("dve", lambda e: e.memset(cc[:, :, :], 0.0), [], [cc])
                            elif i0 > 0:
                                cx.op("dve", lambda e: e.memset(cc[:, 1, 0:i0], 0.0), [], [cc])
                            for x, t in enumerate(pairs[m]):
                                if t["last"]:
                                    continue
                                for i in range(i0, 4):
                                    mm(cx, C[:, 4 * x + i:4 * x + i + 1], sp[:, x, i * 128:(i + 1) * 128], ones[:, :],
                                       True, True, [sp, ones], [C])
                            cx.op("dve", lambda e: e.tensor_tensor(out=cc[:, 1, i0:4], in0=cc[:, 0, i0:4], in1=C[:, i0:4],
                                                                   op=ALU.add), [cc, C], [cc])
                            if not pairs[m][1]["last"]:
                                cx.op("dve", lambda e: e.memset(cn[:, 0, :], 0.0), [], [cn])
                                cx.op("dve", lambda e: e.tensor_tensor(out=cn[:, 0, i0:4], in0=cc[:, 1, i0:4],
                                                                       in1=C[:, 4 + i0:8], op=ALU.add), [cc, C], [cn])

                        def sW(m):
                            t0 = pairs[m][0]["t0"]
                            B, w_, f_ = psA[m % 3], wtb[m % 2], fb[m % 2]
                            act(cx, w_[:, :, t0:512], B[:, :, t0:512], AF.Exp, [B], [w_])
                            act(cx, f_[:, :, :], cbuf[m % 3][:, :, :], AF.Exp, [cbuf[m % 3]], [f_], scale=-1.0)

                        def sPV(m):
                            w_, O, f_ = wtb[m % 2], psO, fb[m % 2]
                            i0 = pairs[m][0]["i0"]
                            for x, t in enumerate(pairs[m]):
                                kb = t["kb"]
                                for i in range(i0, 4):
                                    mm(cx, O[:, 4 * x + i, :], w_[:, x, i * 128:(i + 1) * 128],
                                       vv[:, kb, hh * 64:hh * 64 + 64], True, True, [w_, vv], [O])
                            for x, t in enumerate(pairs[m]):
                                q = t["q"]
                                if t["first"]:
                                    for i in range(4):
                                        if i >= i0:
                                            cx.op("dve", lambda e: e.tensor_copy(out=oacc[i][:, :], in_=O[:, 4 * x + i, :]),
                                                  [O], [oacc[i]])
                                        else:
                                            cx.op("dve", lambda e: e.memset(oacc[i][:, :], 0.0), [], [oacc[i]])
                                else:
                                    for i in range(i0, 4):
                                        dst = ysb[:, 4 * q + i, h * 64:(h + 1) * 64] if t["last"] else oacc[i][:, :]
                                        wr = [ysb] if t["last"] else [oacc[i]]
                                        cx.op("dve", lambda e: e.scalar_tensor_tensor(
                                            out=dst, in0=O[:, 4 * x + i, :], scalar=f_[:, x, i:i + 1], in1=oacc[i][:, :],
                                            op0=ALU.mult, op1=ALU.add), [O, f_, oacc[i]], wr)

                        ok = lambda k: 0 <= k < n
                        for j in range(-2, n + 2):
                            if ok(j):
                                sBC(j)
                            if ok(j + 1):
                                sE(j + 1)
                            if ok(j - 1):
                                sW(j - 1)
                            if ok(j + 2):
                                s1(j + 2)
                            if ok(j + 1):
                                sSP(j + 1)
                            if ok(j - 2):
                                sPV(j - 2)
                cx.barrier()
            with ExitStack() as ph:
                maskD = cx.sb(ph, [128, 8, 512], BF16, "maskD")
                cx.dma("pool", maskD[:, :, :], d["maskD"][:, :, :], writes=[maskD], semb=maskD)
                cx.op("dve", lambda e: e.tensor_scalar(out=maskD[:, :, :], in0=maskD[:, :, :], scalar1=-1.0,
                                                       scalar2=30000.0, op0=ALU.add, op1=ALU.mult), [maskD], [maskD])
                identD = cx.sb(ph, [128, 128], BF16, "identD")
                cx.dma("pool", identD[:, :], d["ident"][:, :], writes=[identD], semb=identD)
                gsub = cx.sb(ph, [128, 128], F32, "gsub")
                cx.dma("sp", gsub[:, :], d["gsub" + sfx][:, :], writes=[gsub], semb=gsub)
                cx.op("dve", lambda e: e.tensor_scalar(out=gsub[:, :], in0=gsub[:, :], scalar1=(1.0 - lam_init),
                                                       scalar2=None, op0=ALU.mult), [gsub], [gsub])
                row = cx.sb(ph, [1, 128], F32, "gqkrow")
                cx.dma("sp", row[:, :], d["gqk_row" + sfx][:, :], writes=[row], semb=row)
                lrow = cx.sb(ph, [1, 256], F32, "lamrow")
                cx.dma("sp", lrow[:, :], d["lam_row" + sfx][:, :], writes=[lrow], semb=lrow)
                sc = cx.sb(ph, [1, 8], F32, "sc")
                prod = cx.sb(ph, [1, 128], F32, "prod")
                ones1 = cx.sb(ph, [1, 128], F32, "ones1")
                cx.op("dve", lambda e: e.memset(ones1[:, :], 1.0), [], [ones1])
                cx.op("dve", lambda e: e.memset(sc[:, :], 0.0), [], [sc])
                cx.op("dve", lambda e: e.reduce_max(out=sc[:, 0:1], in_=row[:, 0:64], axis=AX.X, apply_absolute_value=True), [row], [sc])
                cx.op("dve", lambda e: e.reduce_max(out=sc[:, 1:2], in_=row[:, 64:128], axis=AX.X, apply_absolute_value=True), [row], [sc])
                cx.op("dve", lambda e: e.scalar_tensor_tensor(out=sc[:, 2:3], in0=sc[:, 0:1], scalar=-8.0,
                                                              in1=sc[:, 1:2], op0=ALU.mult, op1=ALU.mult),
                      [sc], [sc])
                cx.op("dve", lambda e: e.tensor_tensor(out=prod[:, 0:64], in0=lrow[:, 0:64], in1=lrow[:, 64:128],
                                                       op=ALU.mult), [lrow], [prod])
                cx.op("dve", lambda e: e.tensor_tensor(out=prod[:, 64:128], in0=lrow[:, 128:192],
                                                       in1=lrow[:, 192:256], op=ALU.mult), [lrow], [prod])
                cx.op("dve", lambda e: e.reduce_sum(out=sc[:, 4:5], in_=prod[:, 0:64], axis=AX.X), [prod], [sc])
                cx.op("dve", lambda e: e.reduce_sum(out=sc[:, 5:6], in_=prod[:, 64:128], axis=AX.X), [prod], [sc])
                act(cx, sc[:, 4:6], sc[:, 4:6], AF.Exp, [sc], [sc])
                cx.op("dve", lambda e: e.scalar_tensor_tensor(out=sc[:, 3:4], in0=sc[:, 4:5], scalar=lam_init,
                                                              in1=sc[:, 5:6], op0=ALU.add, op1=ALU.subtract),
                      [sc], [sc])
                psZ = [[cx.ps(ph, [128, 512], F32, f"psZ{i}{c}") for c in range(2)] for i in range(2)]
                psS = psZ[0][0]
                mm(cx, psS[:, 0:2], ones1[:, :], sc[:, 2:4], True, True, [ones1, sc], [psS])
                negM = cx.sb(ph, [128, 1], F32, "negM")
                lamb = cx.sb(ph, [128, 1], F32, "lamb")
                cx.op("dve", lambda e: e.tensor_copy(out=negM[:, :], in_=psS[:, 0:1]), [psS], [negM])
                cx.op("dve", lambda e: e.tensor_copy(out=lamb[:, :], in_=psS[:, 1:2]), [psS], [lamb])

                ktc = [cx.sb(ph, [68, S], BF16, f"ktd{c}") for c in range(2)]
                qtcs = [[cx.sb(ph, [68, S2], BF16, f"qtd{b_}{c}") for c in range(2)] for b_ in range(2)]
                vas = [cx.sb(ph, [128, NB, 129], BF16, f"va{b_}") for b_ in range(2)]
                for c in range(2):
                    cx.op("dve", lambda e: e.memset(ktc[c][64:68, :], 1.0), [], [ktc[c]])
                    for b_ in range(2):
                        cx.op("dve", lambda e: e.memset(qtcs[b_][c][64:68, :], 1.0), [], [qtcs[b_][c]])
                for b_ in range(2):
                    cx.op("dve", lambda e: e.memset(vas[b_][:, :, 128:129], 1.0), [], [vas[b_]])

                def load_qv(h_):
                    r0_ = 512 + h_ * 128
                    for c in range(2):
                        q_ = qtcs[h_ % 2][c]
                        cx.dma("sp", q_[0:64, :], d["QT"][r0_ + c * 64:r0_ + c * 64 + 64, :], writes=[q_], semb=q_)
                        cx.dma("pool", q_[66:68, :], d["qx"][h_, :, :], writes=[q_], semb=q_)
                    cx.dma("sp", vas[h_ % 2][:, :, 0:128],
                           d["V"][:, r0_:r0_ + 128].rearrange("(b p) c -> p b c", p=128),
                           reads=[], writes=[vas[h_ % 2]], semb=vas[h_ % 2])
                psO = [cx.ps(ph, [128, 512], F32, f"psOd{i}") for i in range(4)]
                pb = [[cx.sb(ph, [128, 512], BF16, f"pb{i}{c}") for c in range(2)] for i in range(2)]
                rl = cx.sb(ph, [128, 4], F32, "rl")
                t1 = cx.sb(ph, [128, 128], F32, "t1")
                ob = cx.sb(ph, [128, 128], F32, "ob")
                junk = cx.sb(ph, [128, 128], F32, "junk")
                ssd = cx.sb(ph, [128, 2], F32, "ssd")

                load_qv(0)
                for h in range(4):
                    r0 = 512 + h * 128
                    for c in range(2):
                        cx.dma("sp", ktc[c][0:64, :], d["KT"][r0 + c * 64:r0 + c * 64 + 64, :], writes=[ktc[c]],
                               semb=ktc[c])
                        cx.dma("pool", ktc[c][64:66, :], d["kx"][h, :, :], writes=[ktc[c]], semb=ktc[c])
                    qtc, va = qtcs[h % 2], vas[h % 2]
                    if h + 1 < 4:
                        load_qv(h + 1)
                    n = len(tl)

                    def s1(k):
                        t = tl[k]
                        t0, kb, q = t["t0"], t["kb"], t["q"]
                        for c in range(2):
                            Z = psZ[k % 2][c]
                            mm(cx, Z[:, t0:512], ktc[c][:, kb * 128:(kb + 1) * 128],
                               qtc[c][:, q * 512 + t0:(q + 1) * 512], True, t["r"] < 0, [ktc[c], qtc[c]], [Z])
                            if t["r"] >= 0:
                                mm(cx, Z[:, t0:512], identD[:, :], maskD[:, t["r"], t0:512], False, True,
                                   [identD, maskD], [Z])

                    def sX(k):
                        t = tl[k]
                        t0 = t["t0"]
                        for c in range(2):
                            Z, p_ = psZ[k % 2][c], pb[k % 2][c]
                            act(cx, p_[:, t0:512], Z[:, t0:512], AF.Exp, [Z, negM], [p_], bias=negM[:, :])

                    def sPV(k):
                        t = tl[k]
                        t0, i0, kb, q = t["t0"], t["i0"], t["kb"], t["q"]
                        for i in range(i0, 4):
                            for c in range(2):
                                p_ = pb[k % 2][c]
                                first_i = (t["u"] == 2 * (3 - i))
                                mm(cx, psO[i][:, c * 256:c * 256 + 129], p_[:, i * 128:(i + 1) * 128],
                                   va[:, kb, :], bool(first_i and c == 0), bool(t["last"]), [p_, va], [psO[i]],
                                   skip_group_check=True)
                        if t["last"]:
                            for i in range(4):
                                O = psO[i]
                                cx.op("dve", lambda e: e.reciprocal(out=rl[:, 0:1], in_=O[:, 128:129]), [O], [rl])
                                cx.op("dve", lambda e: e.reciprocal(out=rl[:, 1:2], in_=O[:, 384:385]), [O], [rl])
                                cx.op("dve", lambda e: e.tensor_tensor(out=rl[:, 2:3], in0=rl[:, 1:2],
                                                                       in1=lamb[:, :], op=ALU.mult), [rl, lamb], [rl])
                                cx.op("dve", lambda e: e.tensor_scalar(out=t1[:, :], in0=O[:, 256:384],
                                                                       scalar1=rl[:, 2:3], scalar2=None,
                                                                       op0=ALU.mult), [O, rl], [t1])
                                cx.op("dve", lambda e: e.scalar_tensor_tensor(
                                    out=ob[:, :], in0=O[:, 0:128], scalar=rl[:, 0:1], in1=t1[:, :],
                                    op0=ALU.mult, op1=ALU.subtract), [O, rl, t1], [ob])
                                cx.op("dve", lambda e: e.tensor_tensor(out=junk[:, :], in0=ob[:, :], in1=ob[:, :],
                                                                       op=ALU.mult), [ob], [junk])
                                cx.op("dve", lambda e: e.reduce_sum(out=ssd[:, 0:1], in_=junk[:, :], axis=AX.X),
                                      [junk], [ssd])
                                act(cx, ssd[:, 1:2], ssd[:, 0:1], AF.Ln, [ssd], [ssd], scale=1.0 / 128, bias=EPS)
                                act(cx, ssd[:, 1:2], ssd[:, 1:2], AF.Exp, [ssd], [ssd], scale=-0.5)
                                cx.op("dve", lambda e: e.scalar_tensor_tensor(
                                    out=ydf[:, 4 * q + i, h * 128:(h + 1) * 128], in0=ob[:, :], scalar=ssd[:, 1:2],
                                    in1=gsub[:, :], op0=ALU.mult, op1=ALU.mult), [ob, ssd, gsub], [ydf])

                    ok = lambda k: 0 <= k < n
                    for j in range(-1, n + 1):
                        if ok(j + 1):
                            s1(j + 1)
                        if ok(j):
                            sX(j)
                        if ok(j - 1):
                            sPV(j - 1)
                cx.barrier()
            if self.debug:
                cx.dma("pool", d["dbg_ysb"].rearrange("(j p) c -> p j c", p=128), ysb[:, :, :], reads=[ysb],
                       writes=[], semb=ysb)
                cx.dma("pool", d["dbg_ydf"].rearrange("(j p) c -> p j c", p=128), ydf[:, :, :], reads=[ydf],
                       writes=[], semb=ydf)
            self.phase_post(L, ph0)

    def phase_post(self, L, ph0):
        cx, nc, d = self.cx, self.nc, self.dr
        sfx = f"_{L}"
        ysb, ydf = self.ysb, self.ydf
        moe = (L % 2 == 1)
        with ExitStack() as ph:
            def loadw(name, K, N, tag):
                kc = K // 128
                w = cx.sb(ph, [128, kc, N], BF16, tag)
                v = d[name + sfx].rearrange("(c p) n -> p c n", p=128)
                for j in range(0, N, 256):
                    cx.dma("pool", w[:, :, j:j + 256], v[:, :, j:j + 256], writes=[w], semb=w)
                return w
            wg = loadw("w_gate", D, 2048, "wg")
            wsb = loadw("w_bsb", 512, D, "wsb")
            wdf = loadw("w_bdf", 512, D, "wdf")
            wo = loadw("w_out", D, D, "wo")
            bg = cx.sb(ph, [1, 2048], BF16, "bg")
            cx.dma("pool", bg[:, :], d["b_gate" + sfx][:, :], writes=[bg], semb=bg)
            onesr = cx.sb(ph, [1, 128], BF16, "onesr")
            cx.op("dve", lambda e: e.memset(onesr[:, :], 1.0), [], [onesr])
            gbc = cx.sb(ph, [128, D], F32, "gbc1")
            cx.dma("sp", gbc[:, :], d["gmix" + sfx][:, :], writes=[gbc], semb=gbc)
            gbc2 = cx.sb(ph, [128, D], F32, "gbc2")
            cx.dma("sp", gbc2[:, :], d["gffn" + sfx][:, :], writes=[gbc2], semb=gbc2)
            ident = cx.sb(ph, [128, 128], BF16, "ident2")
            cx.dma("pool", ident[:, :], d["ident"][:, :], writes=[ident], semb=ident)
            if moe:
                wr = loadw_r = cx.sb(ph, [128, 8, N_EXP], BF16, "wr")
                cx.dma("pool", wr[:, :, :], d["w_r" + sfx].rearrange("(c p) n -> p c n", p=128), writes=[wr], semb=wr)
            xts = [cx.sb(ph, [128, D], F32, f"pxt{i}") for i in range(2)]
            xn = cx.sb(ph, [128, D], BF16, "pxn")
            ss = cx.sb(ph, [128, 1], F32, "pss")
            rt = cx.sb(ph, [128, 1], F32, "prt")
            rs = cx.sb(ph, [128, 1], F32, "prs")
            pT = cx.ps(ph, [128, D], BF16, "ppT")
            hT = cx.sb(ph, [128, 8, 128], BF16, "phT")
            yT = cx.sb(ph, [128, 8, 128], BF16, "pyT")
            mT = cx.sb(ph, [128, 8, 128], BF16, "pmT")
            h2T = cx.sb(ph, [128, 8, 128], BF16, "ph2T")
            gates = cx.sb(ph, [128, 2048], F32, "gates")
            mg = cx.sb(ph, [128, D], F32, "mg")
            mgb = cx.sb(ph, [128, D], BF16, "mgb")
            x1 = [cx.sb(ph, [128, D], F32, f"x1{i}") for i in range(2)]
            pm = [cx.ps(ph, [128, 512], F32, f"ppm{i}") for i in range(4)]
            pr = cx.ps(ph, [128, N_EXP], F32, "ppr")
            lg = cx.sb(ph, [128, N_EXP], F32, "lg")
            l2 = cx.sb(ph, [128, N_EXP], F32, "l2")
            mk = cx.sb(ph, [128, N_EXP], F32, "mk")
            ex = cx.sb(ph, [128, N_EXP], F32, "ex")
            sm = cx.sb(ph, [128, 4], F32, "sm")
            npm = 0
            xnF = [cx.sb(ph, [128, D], BF16, f"pxnF{i}") for i in range(2)]
            hTs_ = [cx.sb(ph, [128, 8, 128], BF16, f"phT{i}") for i in range(2)]
            yTs_ = [cx.sb(ph, [128, 8, 128], BF16, f"pyT{i}") for i in range(2)]
            ssF = [cx.sb(ph, [128, 1], F32, f"pssF{i}") for i in range(2)]
            rtF = [cx.sb(ph, [128, 1], F32, f"prtF{i}") for i in range(2)]
            rsF = [cx.sb(ph, [128, 1], F32, f"prsF{i}") for i in range(2)]
            pTa = cx.ps(ph, [128, D], BF16, "ppTa")

            def front(j):
                k = j % 2
                self.nt_a(cx, self.cur_xown[j * 128:(j + 1) * 128, :], xts[k], ssF[k], rtF[k], rsF[k], xnF[k], gbc)
                self.nt_b(cx, xnF[k], pTa, ident, hTs_[k], 0)
                for c in range(4):
                    cx.op("pe", lambda e: e.transpose(pTa[:, c * 128:(c + 1) * 128], ysb[:, j, c * 128:(c + 1) * 128],
                                                      ident[:, :]), [ysb, ident], [pTa])
                for c in range(4):
                    cx.op("pe", lambda e: e.transpose(pTa[:, (4 + c) * 128:(5 + c) * 128],
                                                      ydf[:, j, c * 128:(c + 1) * 128], ident[:, :]),
                          [ydf, ident], [pTa])
                cx.op("dve", lambda e: e.tensor_copy(out=yTs_[k][:, :, :],
                                                     in_=pTa[:, :].rearrange("p (c t) -> p c t", c=8)), [pTa], [yTs_[k]])

            front(0)
            for j in range(self.NSLOT):
                xt = xts[j % 2]
                xo = x1[j % 2]
                hT = hTs_[j % 2]
                yT = yTs_[j % 2]
                if j + 1 < self.NSLOT:
                    front(j + 1)
                for gi in range(4):
                    P = pm[npm % 4]
                    npm += 1
                    for c in range(8):
                        mm(cx, P[:, :], hT[:, c, :], wg[:, c, gi * 512:(gi + 1) * 512], c == 0, False, [hT, wg], [P])
                    mm(cx, P[:, :], onesr[:, :], bg[:, gi * 512:(gi + 1) * 512], False, True, [onesr, bg], [P])
                    act(cx, gates[:, gi * 512:(gi + 1) * 512], P[:, :], AF.Sigmoid, [P], [gates])
                for nh in range(2):
                    Pa = pm[npm % 4]
                    npm += 1
                    Pb = pm[npm % 4]
                    npm += 1
                    for c in range(4):
                        mm(cx, Pa[:, :], yT[:, c, :], wsb[:, c, nh * 512:(nh + 1) * 512], c == 0, c == 3, [yT, wsb], [Pa])
                    for c in range(4):
                        mm(cx, Pb[:, :], yT[:, 4 + c, :], wdf[:, c, nh * 512:(nh + 1) * 512], c == 0, c == 3,
                           [yT, wdf], [Pb])
                    sl = slice(nh * 512, (nh + 1) * 512)
                    sl2 = slice(1024 + nh * 512, 1024 + (nh + 1) * 512)
                    cx.op("dve", lambda e: e.tensor_tensor(out=mg[:, sl], in0=Pa[:, :], in1=gates[:, sl], op=ALU.mult),
                          [Pa, gates], [mg])
                    cx.op("dve", lambda e: e.tensor_tensor(out=gates[:, sl2], in0=Pb[:, :], in1=gates[:, sl2],
                                                           op=ALU.mult), [Pb, gates], [gates])
                    cx.op("dve", lambda e: e.tensor_tensor(out=mgb[:, sl], in0=mg[:, sl], in1=gates[:, sl2],
                                                           op=ALU.add), [mg, gates], [mgb])
                for c in range(8):
                    cx.op("pe", lambda e: e.transpose(pT[:, c * 128:(c + 1) * 128], mgb[:, c * 128:(c + 1) * 128],
                                                      ident[:, :]), [mgb, ident], [pT])
                cx.op("act", lambda e: e.copy(out=mT[:, :, :], in_=pT[:, :].rearrange("p (c t) -> p c t", c=8)),
                      [pT], [mT])
                for nh in range(2):
                    P = pm[npm % 4]
                    npm += 1
                    for c in range(8):
                        mm(cx, P[:, :], mT[:, c, :], wo[:, c, nh * 512:(nh + 1) * 512], c == 0, c == 7, [mT, wo], [P])
                    sl = slice(nh * 512, (nh + 1) * 512)
                    cx.op("dve", lambda e: e.tensor_tensor(out=xo[:, sl], in0=xt[:, sl], in1=P[:, :], op=ALU.add),
                          [xt, P], [xo])
                cx.dma("sp", d["X1"][j * 128:(j + 1) * 128, :], xo[:, :], reads=[xo], writes=[], semb=xo)
                if self.debug:
                    cx.dma("sp", d["dbg_x1"][j * 128:(j + 1) * 128, :], xo[:, :], reads=[xo], writes=[],
                           semb=xo)
                act(cx, xn[:, :], xo[:, :], AF.Square, [xo], [xn, ss], accum_out=ss[:, :])
                act(cx, rt[:, :], ss[:, :], AF.Sqrt, [ss], [rt], scale=1.0 / D, bias=EPS)
                cx.op("dve", lambda e: e.reciprocal(out=rs[:, :], in_=rt[:, :]), [rt], [rs])
                cx.op("dve", lambda e: e.scalar_tensor_tensor(out=xn[:, :], in0=xo[:, :], scalar=rs[:, :],
                                                              in1=gbc2[:, :], op0=ALU.mult, op1=ALU.mult),
                      [xo, rs, gbc2], [xn])
                for c in range(8):
                    cx.op("pe", lambda e: e.transpose(pT[:, c * 128:(c + 1) * 128], xn[:, c * 128:(c + 1) * 128],
                                                      ident[:, :]), [xn, ident], [pT])
                cx.op("act", lambda e: e.copy(out=h2T[:, :, :], in_=pT[:, :].rearrange("p (c t) -> p c t", c=8)),
                      [pT], [h2T])
                cx.dma("sp", d["H2T"][:, j * 128:(j + 1) * 128].rearrange("(c p) t -> p c t", p=128), h2T[:, :, :],
                       reads=[h2T], writes=[], semb=h2T)
                if moe:
                    rw = self.rw
                    for c in range(8):
                        mm(cx, pr[:, :], h2T[:, c, :], wr[:, c, :], c == 0, c == 7, [h2T, wr], [pr])
                    cx.op("dve", lambda e: e.tensor_copy(out=lg[:, :], in_=pr[:, :]), [pr], [lg])
                    cx.op("dve", lambda e: e.reduce_max(out=sm[:, 0:1], in_=lg[:, :], axis=AX.X), [lg], [sm])
                    cx.dma("sp", d["H2"][j * 128:(j + 1) * 128, :], xn[:, :], reads=[xn], writes=[], semb=xn)
                    oh1A, selA = self.oh1A, self.selA
                    cx.op("dve", lambda e: e.tensor_scalar(out=oh1A[:, j, :], in0=lg[:, :], scalar1=sm[:, 0:1],
                                                           scalar2=None, op0=ALU.is_equal), [lg, sm], [oh1A])
                    cx.op("dve", lambda e: e.scalar_tensor_tensor(out=l2[:, :], in0=oh1A[:, j, :], scalar=NEG_BIG,
                                                                  in1=lg[:, :], op0=ALU.mult, op1=ALU.add),
                          [oh1A, lg], [l2])
                    cx.op("dve", lambda e: e.reduce_max(out=sm[:, 1:2], in_=l2[:, :], axis=AX.X), [l2], [sm])
                    cx.op("dve", lambda e: e.tensor_scalar(out=mk[:, :], in0=lg[:, :], scalar1=sm[:, 1:2],
                                                           scalar2=None, op0=ALU.is_ge), [lg, sm], [mk])
                    cx.op("dve", lambda e: e.tensor_copy(out=selA[:, j, :], in_=mk[:, :]), [mk], [selA])
                    cx.op("dve", lambda e: e.tensor_scalar(out=sm[:, 2:3], in0=sm[:, 0:1], scalar1=-1.0,
                                                           scalar2=None, op0=ALU.mult), [sm], [sm])
                    act(cx, ex[:, :], lg[:, :], AF.Exp, [lg, sm], [ex], bias=sm[:, 2:3])
                    cx.op("dve", lambda e: e.tensor_tensor(out=ex[:, :], in0=ex[:, :], in1=mk[:, :], op=ALU.mult),
                          [ex, mk], [ex])
                    cx.op("dve", lambda e: e.reduce_sum(out=sm[:, 3:4], in_=ex[:, :], axis=AX.X), [ex], [sm])
                    cx.op("dve", lambda e: e.reciprocal(out=sm[:, 3:4], in_=sm[:, 3:4]), [sm], [sm])
                    cx.op("dve", lambda e: e.tensor_scalar(out=rw[:, j, :], in0=ex[:, :], scalar1=sm[:, 3:4],
                                                           scalar2=None, op0=ALU.mult), [ex, sm], [rw])
            cx.barrier()

    def phase_ffn(self, L, last):
        cx, nc, d = self.cx, self.nc, self.dr
        sfx = f"_{L}"
        moe = (L % 2 == 1)
        FS = 512
        passes = []
        if not moe:
            f0 = 0
            while f0 < DFF:
                nf = min(FS, DFF - f0)
                passes.append((d["w_gu" + sfx][:, f0:f0 + nf], d["w_gu" + sfx][:, DFF + f0:DFF + f0 + nf],
                               d["w_dn" + sfx][f0:f0 + nf, :], None, nf))
                f0 += nf
        else:
            for e_ in range(N_EXP):
                for f0 in range(0, DFE, FS):
                    nf = min(FS, DFE - f0)
                    passes.append((d["w1" + sfx][e_, :, f0:f0 + nf], d["w3" + sfx][e_, :, f0:f0 + nf],
                                   d["w2" + sfx][e_, f0:f0 + nf, :], e_, nf))
        GT = min(16, self.NSLOT)
        ngroups = self.NSLOT // GT
        with ExitStack() as ph:
            acc = cx.sb(ph, [128, GT, D], F32, "acc")
            h2 = cx.sb(ph, [128, 8, GT * 128], BF16, "h2")
            w1s = [cx.sb(ph, [128, 8, FS], BF16, f"w1s{i}") for i in range(2)]
            w3s = [cx.sb(ph, [128, 8, FS], BF16, f"w3s{i}") for i in range(2)]
            w2s = [cx.sb(ph, [128, FS // 128, D], BF16, f"w2s{i}") for i in range(2)]
            pg = [cx.ps(ph, [128, 512], F32, f"pg{i}") for i in range(2)]
            pu = [cx.ps(ph, [128, 512], F32, f"pu{i}") for i in range(2)]
            po = [cx.ps(ph, [128, 512], F32, f"po{i}") for i in range(2)]
            sg = [cx.sb(ph, [128, 512], F32, f"sg{i}") for i in range(2)]
            hid = [cx.sb(ph, [128, FS // 128, 512], BF16, f"hid{i}") for i in range(2)]
            rw = self.rw if moe else None
            npass = len(passes)
            if not last:
                xtmp = [cx.sb(ph, [128, D], F32, f"xtmp{i}") for i in range(2)]
                pmk = cx.sb(ph, [128, 2], F32, "pmk")
                cx.dma("sp", pmk[:, :], d["pmask"][:, :], writes=[pmk], semb=pmk)

            def load_pass(pi):
                w1a, w3a, w2a, _, nf = passes[pi]
                k = pi % 2
                fc = nf // 128
                cx.dma("pool", w1s[k][:, :, 0:nf], w1a.rearrange("(c p) n -> p c n", p=128), writes=[w1s[k]], semb=w1s[k])
                cx.dma("pool", w3s[k][:, :, 0:nf], w3a.rearrange("(c p) n -> p c n", p=128), writes=[w3s[k]], semb=w3s[k])
                for c in range(fc):
                    cx.dma("pool", w2s[k][:, c, :], w2a[c * 128:(c + 1) * 128, :], writes=[w2s[k]], semb=w2s[k])

            def group_inputs(g_):
                s0_ = g_ * GT
                cx.dma("sp", h2[:, :, :], d["H2T"][:, s0_ * 128:(s0_ + GT) * 128].rearrange("(c p) t -> p c t", p=128),
                       reads=[], writes=[h2], semb=h2)
                load_pass(0)
                if npass > 1:
                    load_pass(1)

            group_inputs(0)
            for g in range(ngroups):
                s0 = g * GT
                cx.dma("sp", acc[:, :, :], d["X1"][s0 * 128:(s0 + GT) * 128, :].rearrange("(j p) c -> p j c", p=128),
                       reads=[], writes=[acc], semb=acc)
                nst = GT // 4
                units = [(pi, st) for pi in range(npass) for st in range(nst)]

                def gu(n):
                    pi, st = units[n]
                    nf = passes[pi][4]
                    k = pi % 2
                    fc = nf // 128
                    hd = hid[n % 2]
                    for f in range(fc):
                        G_, U_ = pg[f % 2], pu[f % 2]
                        for c in range(8):
                            mm(cx, G_[:, :], w1s[k][:, c, f * 128:(f + 1) * 128], h2[:, c, st * 512:(st + 1) * 512],
                               c == 0, c == 7, [w1s[k], h2], [G_])
                        for c in range(8):
                            mm(cx, U_[:, :], w3s[k][:, c, f * 128:(f + 1) * 128], h2[:, c, st * 512:(st + 1) * 512],
                               c == 0, c == 7, [w3s[k], h2], [U_])
                        s_ = sg[f % 2]
                        act(cx, s_[:, :], G_[:, :], AF.Silu, [G_], [s_])
                        cx.op("dve", lambda e: e.tensor_tensor(out=hd[:, f, :], in0=s_[:, :], in1=U_[:, :],
                                                               op=ALU.mult), [s_, U_], [hd])

                def oo(n):
                    pi, st = units[n]
                    ex_, nf = passes[pi][3], passes[pi][4]
                    k = pi % 2
                    fc = nf // 128
                    hd = hid[n % 2]
                    for u in range(4):
                        j = st * 4 + u
                        for nh in range(2):
                            O = po[nh]
                            for f in range(fc):
                                mm(cx, O[:, :], hd[:, f, u * 128:(u + 1) * 128], w2s[k][:, f, nh * 512:(nh + 1) * 512],
                                   f == 0, f == fc - 1, [hd, w2s[k]], [O])
                            sl = slice(nh * 512, (nh + 1) * 512)
                            if ex_ is None:
                                cx.op("dve", lambda e: e.tensor_tensor(out=acc[:, j, sl], in0=acc[:, j, sl],
                                                                       in1=O[:, :], op=ALU.add), [acc, O], [acc])
                            else:
                                cx.op("dve", lambda e: e.scalar_tensor_tensor(
                                    out=acc[:, j, sl], in0=O[:, :], scalar=rw[:, s0 + j, ex_:ex_ + 1],
                                    in1=acc[:, j, sl], op0=ALU.mult, op1=ALU.add), [O, rw, acc], [acc])

                gu(0)
                for n in range(len(units)):
                    if n + 1 < len(units):
                        gu(n + 1)
                    oo(n)
                    pi, st = units[n]
                    if st == nst - 1 and pi + 2 < npass:
                        load_pass(pi + 2)
                if g + 1 < ngroups:
                    group_inputs(g + 1)
                dst = d["xout"] if last else d["XOWN2"]
                cx.dma("sp", dst[s0 * 128:(s0 + GT) * 128, :].rearrange("(j p) c -> p j c", p=128), acc[:, :, :],
                       reads=[acc], writes=[], semb=acc)
                if not last:
                    for j in range(GT):
                        for pp in range(2):
                            tm = xtmp[(2 * j + pp) % 2]
                            cx.op("dve", lambda e: e.tensor_scalar(out=tm[:, :], in0=acc[:, j, :], scalar1=pmk[:, pp:pp + 1],
                                                                   scalar2=None, op0=ALU.mult), [acc, pmk], [tm])
                            cx.dma("sp", d["XG2"][pp, (s0 + j) * 128:(s0 + j + 1) * 128, :], tm[:, :], reads=[tm],
                                   writes=[], semb=tm)
                    P_ = cx.E["pool"]
                    for tm in xtmp:
                        for s_ in (tm.dsem or {}).values():
                            if P_.seen.get(id(s_), 0) < s_.total:
                                P_.eng.wait_ge(s_.h, s_.total)
                                P_.seen[id(s_)] = s_.total
                    xg = d["XG2"].rearrange("a r c -> (a r) c")
                    xf = d["XF2"].rearrange("a r c -> (a r) c")
                    for pp in range(2):
                        for r0 in range(0, GT * 128, 1024):
                            rr = pp * self.S2 + s0 * 128 + r0
                            nrows = min(1024, GT * 128 - r0)
                            ins = nc.gpsimd.collective_compute(
                                "AllReduce", ALU.add,
                                replica_groups=[[2 * i, 2 * i + 1] for i in range(self.ncores // 2)],
                                ins=[xg[rr:rr + nrows, :]], outs=[xf[rr:rr + nrows, :]])
                            ins.then_inc(self.ccs.h)
                            self.ccs.total += 1
                    self.tXF.lw = (self.ccs, self.ccs.total)
            cx.barrier()


    def idma(self, out, in_, out_off, in_off, reads, writes, semb):
        cx = self.cx
        Q = cx.E["pool"]
        cx._sync(Q, reads, writes)
        ds = cx.dsem_for(semb, "sw")
        ins = Q.eng.indirect_dma_start(out=out, out_offset=out_off, in_=in_, in_offset=in_off)
        ds.total += 16
        ins.then_inc(ds.h, 16)
        cx._reg((ds, ds.total), reads, writes)

    def phase_moe_sparse(self, L):
        cx, nc, d = self.cx, self.nc, self.dr
        sfx = f"_{L}"
        NS, E, NBLK, NSL = self.NSLOT, N_EXP, self.NBLK, 7
        I32 = mybir.dt.int32
        IOA = bass.IndirectOffsetOnAxis
        selA, oh1A, rw = self.selA, self.oh1A, self.rw
        with ExitStack() as ph:
            pT = cx.ps(ph, [128, D], BF16, "mpT")
            pg = [cx.ps(ph, [128, 512], F32, f"mpg{i}") for i in range(2)]
            pu = [cx.ps(ph, [128, 512], F32, f"mpu{i}") for i in range(2)]
            po = [cx.ps(ph, [128, 512], F32, f"mpo{i}") for i in range(2)]
            ident = cx.sb(ph, [128, 128], BF16, "mident")
            cx.dma("pool", ident[:, :], d["ident"][:, :], writes=[ident], semb=ident)
            ltri = cx.sb(ph, [128, 128], BF16, "ltri")
            cx.dma("pool", ltri[:, :], d["ltri"][:, :], writes=[ltri], semb=ltri)
            onesq = cx.sb(ph, [128, 128], BF16, "onesq")
            cx.op("dve", lambda e: e.memset(onesq[:, :], 1.0), [], [onesq])
            s128p = cx.sb(ph, [128, 2 * NSL], F32, "s128p")
            cx.dma("sp", s128p[:, :], d["s128p"][:, :], writes=[s128p], semb=s128p)
            NE = NS * E
            selb = cx.sb(ph, [128, NE], BF16, "selb")
            cx.op("dve", lambda e: e.tensor_copy(out=selb[:, :], in_=selA[:, :, :].rearrange("p j e -> p (j e)")),
                  [selA], [selb])
            mm(cx, pg[0][:, 0:NE], ltri[:, :], selb[:, :], True, True, [ltri, selb], [pg[0]])
            mm(cx, pu[0][:, 0:NE], onesq[:, :], selb[:, :], True, True, [onesq, selb], [pu[0]])
            pre = cx.sb(ph, [128, NS, E], F32, "pre")
            tot = cx.sb(ph, [128, NS, E], F32, "tot")
            cum = cx.sb(ph, [128, NS, E], F32, "cum")
            cx.op("dve", lambda e: e.tensor_copy(out=pre[:, :, :].rearrange("p j e -> p (j e)"), in_=pg[0][:, 0:NE]),
                  [pg[0]], [pre])
            cx.op("dve", lambda e: e.tensor_copy(out=tot[:, :, :].rearrange("p j e -> p (j e)"), in_=pu[0][:, 0:NE]),
                  [pu[0]], [tot])
            cx.op("dve", lambda e: e.memset(cum[:, 0, :], 0.0), [], [cum])
            for j in range(1, NS):
                cx.op("dve", lambda e: e.tensor_tensor(out=cum[:, j, :], in0=cum[:, j - 1, :], in1=tot[:, j - 1, :],
                                                       op=ALU.add), [cum, tot], [cum])
            sc = cx.sb(ph, [128, 6, E], F32, "msc")
            sci = cx.sb(ph, [128, E], I32, "msci")
            cx.op("dve", lambda e: e.tensor_tensor(out=sc[:, 0, :], in0=cum[:, NS - 1, :], in1=tot[:, NS - 1, :],
                                                   op=ALU.add), [cum, tot], [sc])
            cx.op("dve", lambda e: e.tensor_scalar(out=sc[:, 1, :], in0=sc[:, 0, :], scalar1=511.0, scalar2=None,
                                                   op0=ALU.add), [sc], [sc])
            cx.op("dve", lambda e: e.tensor_copy(out=sci[:, :], in_=sc[:, 1, :]), [sc], [sci])
            cx.op("dve", lambda e: e.tensor_scalar(out=sci[:, :], in0=sci[:, :], scalar1=9, scalar2=9,
                                                   op0=ALU.arith_shift_right, op1=ALU.logical_shift_left),
                  [sci], [sci])
            cx.op("dve", lambda e: e.tensor_copy(out=sc[:, 2, :], in_=sci[:, :]), [sci], [sc])
            cx.op("dve", lambda e: e.memset(sc[:, 3, 0:1], 0.0), [], [sc])
            for e_ in range(1, E):
                cx.op("dve", lambda e: e.tensor_tensor(out=sc[:, 3, e_:e_ + 1], in0=sc[:, 3, e_ - 1:e_],
                                                       in1=sc[:, 2, e_ - 1:e_], op=ALU.add), [sc], [sc])
            cx.op("dve", lambda e: e.tensor_tensor(out=sc[:, 4, :], in0=sc[:, 3, :], in1=sc[:, 2, :], op=ALU.add),
                  [sc], [sc])
            cx.op("dve", lambda e: e.tensor_tensor(out=cum[:, :, :], in0=cum[:, :, :], in1=pre[:, :, :], op=ALU.add),
                  [cum, pre], [cum])
            for j in range(NS):
                cx.op("dve", lambda e: e.tensor_tensor(out=cum[:, j, :], in0=cum[:, j, :], in1=sc[:, 3, :],
                                                       op=ALU.add), [cum, sc], [cum])
            dest = cum
            oh2 = pre
            cx.op("dve", lambda e: e.tensor_tensor(out=oh2[:, :, :], in0=selA[:, :, :], in1=oh1A[:, :, :],
                                                   op=ALU.subtract), [selA, oh1A], [oh2])
            prod = tot
            dr = cx.sb(ph, [128, 4, NS], F32, "mdr")
            for qi, (ma, va_) in enumerate(((oh1A, dest), (oh2, dest), (oh1A, rw), (oh2, rw))):
                cx.op("dve", lambda e: e.tensor_tensor(out=prod[:, :, :], in0=ma[:, :, :], in1=va_[:, :, :],
                                                       op=ALU.mult), [ma, va_], [prod])
                cx.op("dve", lambda e: e.reduce_sum(out=dr[:, qi, :], in_=prod[:, :, :], axis=AX.X), [prod], [dr])
            di = cx.sb(ph, [128, 2, NS], I32, "mdi")
            cx.op("dve", lambda e: e.tensor_scalar(out=dr[:, 0:2, :], in0=dr[:, 0:2, :], scalar1=float(NBLK * 512 - 1),
                                                   scalar2=None, op0=ALU.min), [dr], [dr])
            cx.op("dve", lambda e: e.tensor_copy(out=di[:, :, :], in_=dr[:, 0:2, :]), [dr], [di])
            beF = cx.sb(ph, [128, NBLK], F32, "beF")
            for b in range(NBLK):
                cx.op("dve", lambda e: e.tensor_scalar(out=sc[:, 5, :], in0=sc[:, 4, :], scalar1=float(512 * b),
                                                       scalar2=None, op0=ALU.is_le), [sc], [sc])
                cx.op("dve", lambda e: e.reduce_sum(out=beF[:, b:b + 1], in_=sc[:, 5, :], axis=AX.X), [sc], [beF])
            cx.op("dve", lambda e: e.tensor_scalar(out=beF[:, :], in0=beF[:, :], scalar1=float(E - 1),
                                                   scalar2=float(2 * NSL * 128), op0=ALU.min, op1=ALU.mult), [beF], [beF])
            idxF = cx.sb(ph, [128, NBLK * 2 * NSL], F32, "idxF")
            for b in range(NBLK):
                cx.op("dve", lambda e: e.tensor_scalar(out=idxF[:, b * 2 * NSL:(b + 1) * 2 * NSL], in0=s128p[:, :],
                                                       scalar1=beF[:, b:b + 1], scalar2=None, op0=ALU.add),
                      [s128p, beF], [idxF])
            idxI = cx.sb(ph, [128, NBLK * 2 * NSL], I32, "idxI")
            cx.op("dve", lambda e: e.tensor_copy(out=idxI[:, :], in_=idxF[:, :]), [idxF], [idxI])

            xsb = [cx.sb(ph, [128, 4, D], BF16, f"xsb{i}") for i in range(2)]
            cx.op("dve", lambda e: e.memset(xsb[0][:, :, :], 0.0), [], [xsb[0]])
            for b in range(NBLK):
                cx.dma("sp", d["XS"][b * 512:(b + 1) * 512, :].rearrange("(u p) c -> p u c", p=128), xsb[0][:, :, :],
                       reads=[xsb[0]], writes=[], semb=xsb[0])
            cx.barrier(release=False)
            hrow = [cx.sb(ph, [128, D], BF16, f"hrow{i}") for i in range(2)]
            for j in range(NS):
                hr = hrow[j % 2]
                cx.dma("sp", hr[:, :], d["H2"][j * 128:(j + 1) * 128, :], writes=[hr], semb=hr)
                for k in range(2):
                    self.idma(d["XS"][:, :], hr[:, :], IOA(ap=di[:, k, j:j + 1], axis=0), None, [hr, di], [], hr)
            cx.barrier(release=False)

            xT = [cx.sb(ph, [128, 8, 512], BF16, f"mxT{i}") for i in range(2)]
            w1s = [cx.sb(ph, [128, 8, 512], BF16, f"mw1{i}") for i in range(3)]
            w3s = [cx.sb(ph, [128, 8, 512], BF16, f"mw3{i}") for i in range(3)]
            w2s = [cx.sb(ph, [128, 4, D], BF16, f"mw2{i}") for i in range(3)]
            sg = [cx.sb(ph, [128, 512], F32, f"msg{i}") for i in range(2)]
            hid = [cx.sb(ph, [128, 4, 512], BF16, f"mhid{i}") for i in range(2)]
            accb = [cx.sb(ph, [128, 4, D], F32, f"macc{i}") for i in range(2)]
            cmb = [[cx.sb(ph, [128, D], F32, f"mcmb{a}{i}") for i in range(2)] for a in range(3)]
            units = [(b, s_) for b in range(NBLK) for s_ in range(NSL)]

            def loadw(n):
                k = n % 3
                for hf in range(2):
                    col = idxI[:, 2 * n + hf:2 * n + hf + 1]
                    self.idma(w1s[k][:, 4 * hf:4 * hf + 4, :].rearrange("p c n -> p (c n)"), d["w1r" + sfx][:, :], None,
                              IOA(ap=col, axis=0), [idxI], [w1s[k]], w1s[k])
                    self.idma(w3s[k][:, 4 * hf:4 * hf + 4, :].rearrange("p c n -> p (c n)"), d["w3r" + sfx][:, :], None,
                              IOA(ap=col, axis=0), [idxI], [w3s[k]], w3s[k])
                    self.idma(w2s[k][:, 2 * hf:2 * hf + 2, :].rearrange("p c n -> p (c n)"), d["w2r" + sfx][:, :], None,
                              IOA(ap=col, axis=0), [idxI], [w2s[k]], w2s[k])

            def prep(b):
                xs = xsb[b % 2]
                cx.dma("sp", xs[:, :, :], d["XS"][b * 512:(b + 1) * 512, :].rearrange("(u p) c -> p u c", p=128),
                       writes=[xs], semb=xs)
                for u in range(4):
                    for c in range(8):
                        cx.op("pe", lambda e: e.transpose(pT[:, c * 128:(c + 1) * 128], xs[:, u, c * 128:(c + 1) * 128],
                                                          ident[:, :]), [xs, ident], [pT])
                    cx.op("act", lambda e: e.copy(out=xT[b % 2][:, :, u * 128:(u + 1) * 128],
                                                  in_=pT[:, :].rearrange("p (c t) -> p c t", c=8)), [pT], [xT[b % 2]])

            def gu(n):
                b, s_ = units[n]
                k = n % 3
                hd = hid[n % 2]
                x_ = xT[b % 2]
                for f in range(4):
                    G_, U_ = pg[f % 2], pu[f % 2]
                    for c in range(8):
                        mm(cx, G_[:, :], w1s[k][:, c, f * 128:(f + 1) * 128], x_[:, c, :], c == 0, c == 7,
                           [w1s[k], x_], [G_])
                    for c in range(8):
                        mm(cx, U_[:, :], w3s[k][:, c, f * 128:(f + 1) * 128], x_[:, c, :], c == 0, c == 7,
                           [w3s[k], x_], [U_])
                    s2 = sg[f % 2]
                    act(cx, s2[:, :], G_[:, :], AF.Silu, [G_], [s2])
                    cx.op("dve", lambda e: e.tensor_tensor(out=hd[:, f, :], in0=s2[:, :], in1=U_[:, :], op=ALU.mult),
                          [s2, U_], [hd])

            def oo(n):
                b, s_ = units[n]
                k = n % 3
                hd = hid[n % 2]
                ac = accb[b % 2]
                for u in range(4):
                    for nh in range(2):
                        O = po[nh]
                        for f in range(4):
                            mm(cx, O[:, :], hd[:, f, u * 128:(u + 1) * 128], w2s[k][:, f, nh * 512:(nh + 1) * 512],
                               f == 0, f == 3, [hd, w2s[k]], [O])
                        sl = slice(nh * 512, (nh + 1) * 512)
                        if s_ == 0:
                            cx.op("dve", lambda e: e.tensor_copy(out=ac[:, u, sl], in_=O[:, :]), [O], [ac])
                        else:
                            cx.op("dve", lambda e: e.tensor_tensor(out=ac[:, u, sl], in0=ac[:, u, sl], in1=O[:, :],
                                                                   op=ALU.add), [ac, O], [ac])
                if s_ == NSL - 1:
                    cx.dma("sp", d["YS"][b * 512:(b + 1) * 512, :].rearrange("(u p) c -> p u c", p=128), ac[:, :, :],
                           reads=[ac], writes=[], semb=ac)

            loadw(0)
            loadw(1)
            loadw(2)
            prep(0)
            gu(0)
            for n in range(len(units)):
                b, s_ = units[n]
                if s_ == 2 and b + 1 < NBLK:
                    prep(b + 1)
                if n + 1 < len(units):
                    gu(n + 1)
                oo(n)
                if n + 3 < len(units):
                    loadw(n + 3)
            cx.barrier(release=False)

            for j in range(NS):
                a_ = T(accb[0].t[:, (j % 2) * 2, :]) if False else None
                a_, b_, x_ = cmb[0][j % 2], cmb[1][j % 2], cmb[2][j % 2]
                cx.dma("sp", x_[:, :], d["X1"][j * 128:(j + 1) * 128, :], writes=[x_], semb=x_)
                self.idma(a_[:, :], d["YS"][:, :], None, IOA(ap=di[:, 0, j:j + 1], axis=0), [di], [a_], a_)
                self.idma(b_[:, :], d["YS"][:, :], None, IOA(ap=di[:, 1, j:j + 1], axis=0), [di], [b_], b_)
                cx.op("dve", lambda e: e.scalar_tensor_tensor(out=x_[:, :], in0=a_[:, :], scalar=dr[:, 2, j:j + 1],
                                                              in1=x_[:, :], op0=ALU.mult, op1=ALU.add),
                      [a_, dr, x_], [x_])
                cx.op("dve", lambda e: e.scalar_tensor_tensor(out=x_[:, :], in0=b_[:, :], scalar=dr[:, 3, j:j + 1],
                                                              in1=x_[:, :], op0=ALU.mult, op1=ALU.add),
                      [b_, dr, x_], [x_])
                cx.dma("sp", d["xout"][j * 128:(j + 1) * 128, :], x_[:, :], reads=[x_], writes=[], semb=x_)
            cx.barrier()

def slot_blocks(p, nslot):
    return [4 * (j // 2) + GSLOT[p][j % 2] for j in range(nslot)]


def gather_own(xb, p):
    S = xb.shape[0]
    blocks = slot_blocks(p, S // 256)
    return np.ascontiguousarray(np.concatenate([xb[g * 128:(g + 1) * 128] for g in blocks], axis=0))


def layer_inputs(inp, L):
    f = lambda a: np.ascontiguousarray(np.asarray(a, dtype=np.float32))
    sfx = f"_{L}"
    m = {}
    m["gmix" + sfx] = f(np.broadcast_to(inp["norm_mix_g"][L][None, :], (128, D)))
    m["w_in" + sfx] = f(inp["w_in"][L])
    gq, gk = inp["diff_q_norm_g"][L], inp["diff_k_norm_g"][L]
    m["gq" + sfx] = f(np.concatenate([gq, gq])[:, None])
    m["gk" + sfx] = f(np.concatenate([gk, gk])[:, None])
    m["gqk_row" + sfx] = f(np.concatenate([gq, gk])[None, :])
    m["lam_row" + sfx] = f(inp["diff_lambda"][L].reshape(1, 256))
    m["gsub" + sfx] = f(np.broadcast_to(inp["diff_subln_g"][L][None, :], (128, 128)))
    m["w_gate" + sfx] = f(inp["w_gate"][L])
    m["b_gate" + sfx] = f(inp["b_gate"][L][None, :])
    m["w_bsb" + sfx] = f(inp["w_branch_sb"][L])
    m["w_bdf" + sfx] = f(inp["w_branch_diff"][L])
    m["w_out" + sfx] = f(inp["w_out"][L])
    m["gffn" + sfx] = f(np.broadcast_to(inp["norm_ffn_g"][L][None, :], (128, D)))
    j = L // 2
    if L % 2 == 0:
        m["w_gu" + sfx] = f(inp["ffn_w_gate_up"][j])
        m["w_dn" + sfx] = f(inp["ffn_w_down"][j])
    else:
        m["w_r" + sfx] = f(inp["moe_w_router"][j])
        w1 = np.asarray(inp["moe_w1"][j], dtype=np.float32)
        w3 = np.asarray(inp["moe_w3"][j], dtype=np.float32)
        w2 = np.asarray(inp["moe_w2"][j], dtype=np.float32)
        rl13 = lambda w: np.ascontiguousarray(
            w.reshape(N_EXP, 2, 4, 128, 7, 512).transpose(0, 4, 1, 3, 2, 5)).reshape(N_EXP * 14 * 128, 2048)
        m["w1r" + sfx] = rl13(w1)
        m["w3r" + sfx] = rl13(w3)
        m["w2r" + sfx] = np.ascontiguousarray(
            w2.reshape(N_EXP, 7, 2, 2, 128, D).transpose(0, 1, 2, 4, 3, 5)).reshape(N_EXP * 14 * 128, 2048)
    return m


_PROG_CACHE = {}


def run_layers(x, inp, layers, debug=False):
    B, S, _ = x.shape
    ncores = 2 * B
    key = (S, tuple(layers), debug)
    if key not in _PROG_CACHE:
        pr = Prog(S, layers, debug, ncores)
        pr.build()
        _PROG_CACHE[key] = pr
    pr = _PROG_CACHE[key]
    in_maps = []
    lw = {}
    for L in layers:
        lw.update(layer_inputs(inp, L))
    for core in range(ncores):
        b, p = core // 2, core % 2
        m = dict(lw)
        m["xfull"] = np.ascontiguousarray(x[b])
        m["xown"] = gather_own(x[b], p)
        hc = host_consts(p, pr.U)
        if not getattr(pr, "has_moe", False):
            hc.pop("ltri"); hc.pop("s128p")
        m.update(hc)
        if len(layers) > 1:
            pm = np.zeros((128, 2), np.float32)
            pm[:, p] = 1.0
            m["pmask"] = pm
        in_maps.append(m)
    res = run_bass_kernel_spmd(pr.nc, in_maps, core_ids=list(range(ncores)))
    out = np.zeros((B, S, D), np.float32)
    dbg = []
    for core in range(ncores):
        b, p = core // 2, core % 2
        r = res.results[core]
        xo = r["xout"]
        for j, g in enumerate(slot_blocks(p, S // 256)):
            out[b, g * 128:(g + 1) * 128] = xo[j * 128:(j + 1) * 128]
        dbg.append(r)
    return out, dbg


def kernel(**inputs):
    inp = {k: np.asarray(v) for k, v in inputs.items()}
    x = np.asarray(inp["x"], dtype=np.float32)
    depth = inp["w_in"].shape[0]
    out, _ = run_layers(x, inp, list(range(depth)))
    return out
```

```python
import math
from contextlib import ExitStack

import numpy as np
import concourse.bass as bass
import concourse.mybir as mybir
from concourse.bass_utils import run_bass_kernel_spmd

F32 = mybir.dt.float32
BF16 = mybir.dt.bfloat16
AF = mybir.ActivationFunctionType
ALU = mybir.AluOpType
AX = mybir.AxisListType

D = 1024
EPS = 1e-6
GSLOT = [[0, 3], [1, 2]]
N_EXP = 8
DFF = 2816
DFE = 3584
NEG_BIG = -1.0e30


class Sem:
    def __init__(self, h, is_dma):
        self.h = h
        self.total = 0
        self.is_dma = is_dma


class T:
    def __init__(self, t):
        self.t = t
        self.lw = None
        self.rd = {}
        self.dsem = None

    def __getitem__(self, idx):
        return self.t[idx]


class Eng:
    def __init__(self, name, eng, sem):
        self.name = name
        self.eng = eng
        self.sem = sem
        self.seen = {}


class Cx:
    def __init__(self, nc, stack):
        self.nc = nc
        self.stack = stack
        self.E = {}
        self.allsems = []
        for name, eng in (("pe", nc.tensor), ("act", nc.scalar), ("dve", nc.vector),
                          ("pool", nc.gpsimd), ("sp", nc.sync)):
            s = Sem(stack.enter_context(nc.semaphore("s_" + name)), False)
            self.allsems.append(s)
            self.E[name] = Eng(name, eng, s)
        self.free_dsems = {}
        self.live_dsems = []
        self.n_dsem = 0
        self.rr = 0

    def sb(self, ph, shape, dt, name=None):
        self.rr += 1
        return T(ph.enter_context(self.nc.sbuf_tensor(f"sb{self.rr}_{name or 't'}", list(shape), dt)))

    def ps(self, ph, shape, dt=F32, name=None):
        self.rr += 1
        return T(ph.enter_context(self.nc.psum_tensor(f"ps{self.rr}_{name or 'p'}", list(shape), dt)))

    def dsem_for(self, tb, kind="hw"):
        if tb.dsem is None:
            tb.dsem = {}
        if kind not in tb.dsem:
            fl = self.free_dsems.setdefault(kind, [])
            if fl:
                s = fl.pop()
            else:
                self.n_dsem += 1
                s = Sem(self.stack.enter_context(self.nc.semaphore(f"d{self.n_dsem}")), True)
                self.allsems.append(s)
            self.live_dsems.append((kind, s))
            tb.dsem[kind] = s
        return tb.dsem[kind]

    def release_phase(self):
        for kind, s_ in self.live_dsems:
            self.free_dsems.setdefault(kind, []).append(s_)
        self.live_dsems = []

    def _sync(self, E, reads, writes):
        need = {}

        def add(tk):
            if tk is None:
                return
            s, v = tk
            if s.is_dma:
                v = s.total
            if s is E.sem and E.name in ("pe", "sp"):
                return
            cur = need.get(id(s))
            if cur is None or cur[1] < v:
                need[id(s)] = (s, v)

        for b in reads:
            add(b.lw)
        for b in writes:
            add(b.lw)
            for tk in b.rd.values():
                add(tk)
        for s, v in need.values():
            if E.seen.get(id(s), 0) >= v:
                continue
            E.eng.wait_ge(s.h, v)
            E.seen[id(s)] = v

    def _reg(self, tk, reads, writes):
        s = tk[0]
        for b in reads:
            b.rd[id(s)] = tk
        for b in writes:
            b.lw = tk
            b.rd = {}

    def op(self, en, fn, reads=(), writes=()):
        E = self.E[en]
        self._sync(E, reads, writes)
        ins = fn(E.eng)
        E.sem.total += 1
        ins.then_inc(E.sem.h, 1)
        self._reg((E.sem, E.sem.total), reads, writes)

    def dma(self, qn, out, in_, reads=(), writes=(), semb=None, **kw):
        Q = self.E[qn]
        self._sync(Q, reads, writes)
        ds = self.dsem_for(semb, "sw" if qn == "pool" else "hw")
        ins = Q.eng.dma_start(out=out, in_=in_, **kw)
        ds.total += 16
        ins.then_inc(ds.h, 16)
        self._reg((ds, ds.total), reads, writes)

    def barrier(self, release=True):
        self._barrier()
        if release:
            self.release_phase()

    def _barrier(self):
        for E in self.E.values():
            for s in self.allsems:
                if s.total == 0:
                    continue
                if E.seen.get(id(s), 0) >= s.total:
                    continue
                E.eng.wait_ge(s.h, s.total)
                E.seen[id(s)] = s.total


def act(cx, out, in_, func, reads, writes, **kw):
    cx.op("act", lambda e: e.activation(out=out, in_=in_, func=func, **kw), reads, writes)


def mm(cx, out, lhsT, rhs, start, stop, reads, writes, **kw):
    cx.op("pe", lambda e: e.matmul(out, lhsT=lhsT, rhs=rhs, start=start, stop=stop, **kw), reads, writes)


def core_gi(p):
    return [GSLOT[p][0], GSLOT[p][1], 4 + GSLOT[p][0], 4 + GSLOT[p][1]]


def host_consts(p, U):
    gi = core_gi(p)
    s = np.arange(128)[:, None]
    t = np.arange(128)[None, :]
    maskS = np.zeros((128, 8, 512), np.float32)
    maskD = np.zeros((128, 8, 512), np.float32)
    for r in range(8):
        for i in range(4):
            sg = r * 128 + s
            tg = gi[i] * 128 + t
            maskS[:, r, i * 128:(i + 1) * 128] = (sg < tg)
            maskD[:, r, i * 128:(i + 1) * 128] = (sg <= tg)
    j = np.arange(128)[:, None]
    negU = -(j >= np.arange(128)[None, :]).astype(np.float32)
    ident = np.eye(128, dtype=np.float32)
    bd = np.zeros((128, 128), np.float32)
    bd[:64, :64] = 1.0
    bd[64:, 64:] = 1.0
    S = U * 128
    nslot = S // 256
    kx = np.zeros((4, 2, S), np.float32)
    qx = np.zeros((4, 2, S // 2), np.float32)
    sidx = np.arange(S)
    tblk = np.repeat(np.array([4 * (j // 2) + GSLOT[p][j % 2] for j in range(nslot)]), 128)
    tloc = np.tile(np.arange(128), nslot)
    for h in range(4):
        m = 2.0 ** (-8.0 * (h + 1) / 4)
        kx[h, 0] = m * 128.0 * (sidx // 128)
        kx[h, 1] = m * (sidx % 128)
        qx[h, 0] = -m * 128.0 * tblk
        qx[h, 1] = -m * tloc
    ltri = (np.arange(128)[:, None] < np.arange(128)[None, :]).astype(np.float32)
    s128p = (np.arange(14)[None, :] * 128.0 + np.arange(128)[:, None]).astype(np.float32)
    return dict(maskS=maskS, maskD=maskD, negU=negU, ident=ident, bd=bd, kx=kx, qx=qx, ltri=ltri, s128p=s128p)


class Prog:
    def __init__(self, S, layers, debug=False, ncores=8):
        self.ncores = ncores
        self.S = S
        self.S2 = S // 2
        self.NQ = S // 1024
        self.NB = S // 128
        self.NSLOT = S // 256
        self.U = self.NB
        self.layers = layers
        self.debug = debug
        self.nc = bass.Bass("TRN2", target_bir_lowering=False)
        self.dr = {}

    def din(self, name, shape, dt=F32):
        self.dr[name] = self.nc.dram_tensor(name, list(shape), dt, kind="ExternalInput").ap()
        return self.dr[name]

    def dout(self, name, shape, dt=F32):
        self.dr[name] = self.nc.dram_tensor(name, list(shape), dt, kind="ExternalOutput").ap()
        return self.dr[name]

    def dscr(self, name, shape, dt):
        self.dr[name] = self.nc.dram_tensor(name, list(shape), dt).ap()
        return self.dr[name]

    def build(self):
        S, S2 = self.S, self.S2
        d = self.dr
        self.din("xfull", [S, D])
        self.din("xown", [S2, D])
        for nm, shp in (("maskS", [128, 8, 512]), ("maskD", [128, 8, 512]), ("negU", [128, 128]),
                        ("ident", [128, 128]), ("bd", [128, 128]), ("kx", [4, 2, S]),
                        ("qx", [4, 2, S2])):
            self.din(nm, shp)
        for L in self.layers:
            sfx = f"_{L}"
            self.din("gmix" + sfx, [128, D])
            self.din("w_in" + sfx, [D, 3072])
            self.din("gq" + sfx, [128, 1])
            self.din("gk" + sfx, [128, 1])
            self.din("gqk_row" + sfx, [1, 128])
            self.din("lam_row" + sfx, [1, 256])
            self.din("gsub" + sfx, [128, 128])
            self.din("w_gate" + sfx, [D, 2048])
            self.din("b_gate" + sfx, [1, 2048])
            self.din("w_bsb" + sfx, [512, D])
            self.din("w_bdf" + sfx, [512, D])
            self.din("w_out" + sfx, [D, D])
            self.din("gffn" + sfx, [128, D])
            if L % 2 == 0:
                self.din("w_gu" + sfx, [D, 2 * DFF])
                self.din("w_dn" + sfx, [DFF, D])
            else:
                self.din("w_r" + sfx, [D, N_EXP])
                self.din("w1r" + sfx, [N_EXP * 14 * 128, 2048])
                self.din("w3r" + sfx, [N_EXP * 14 * 128, 2048])
                self.din("w2r" + sfx, [N_EXP * 14 * 128, 2048])
                self.has_moe = True
        self.dout("xout", [S2, D])
        if self.debug:
            self.dout("dbg_ysb", [S2, 512])
            self.dout("dbg_ydf", [S2, 512])
            self.dout("dbg_x1", [S2, D])
        self.dscr("KT", [D, S], BF16)
        self.dscr("QT", [D, S2], BF16)
        self.dscr("V", [S, D], BF16)
        self.dscr("X1", [S2, D], F32)
        self.dscr("H2T", [D, S2], BF16)
        if getattr(self, "has_moe", False):
            self.NBLK = S2 // 256 + 8
            self.din("ltri", [128, 128])
            self.din("s128p", [128, 14])
            self.dscr("H2", [S2, D], BF16)
            self.dscr("XS", [self.NBLK * 512, D], BF16)
            self.dscr("YS", [self.NBLK * 512, D], F32)
        if len(self.layers) > 1:
            self.dscr("XOWN2", [S2, D], F32)
            self.dscr("XG2", [2, S2, D], F32)
            self.dscr("XF2", [2, S2, D], F32)
            self.din("pmask", [128, 2])

        nc = self.nc
        with ExitStack() as stack:
            cx = Cx(nc, stack)
            self.cx = cx
            self.tKT, self.tQT, self.tV, self.tX1, self.tH2T = T(None), T(None), T(None), T(None), T(None)
            self.tXO = T(None)
            nl = len(self.layers)
            self.tXF = T(None)
            for li, L in enumerate(self.layers):
                last = (li == nl - 1)
                self.first_layer = (li == 0)
                if not last:
                    self.ccs = Sem(stack.enter_context(nc.semaphore(f"cc{li}")), False)
                if li == 0:
                    kv_row = lambda g: d["xfull"][g * 128:(g + 1) * 128, :]
                    xown = d["xown"]
                else:
                    def kv_row(g):
                        pp = 0 if (g % 4) in (0, 3) else 1
                        j = 2 * (g // 4) + (0 if (g % 4) in (0, 1) else 1)
                        return d["XF2"][pp, j * 128:(j + 1) * 128, :]
                    xown = d["XOWN2"]
                self.cur_xown = xown
                with ExitStack() as lst:
                    self.lst = lst
                    if L % 2 == 1:
                        self.rw = cx.sb(lst, [128, self.NSLOT, N_EXP], F32, "rw")
                        self.selA = cx.sb(lst, [128, self.NSLOT, N_EXP], F32, "selA")
                        self.oh1A = cx.sb(lst, [128, self.NSLOT, N_EXP], F32, "oh1A")
                    self.phase_proj(L, kv_row, xown)
                    cx.barrier()
                    self.phase_attn(L)
                    cx.barrier()
                    if L % 2 == 1:
                        assert last
                        self.phase_moe_sparse(L)
                    else:
                        self.phase_ffn(L, last)
                    cx.barrier()
        return nc

    def nt_a(self, cx, row_ap, xt, ss, rt, rstd, xn, gbc, extra_reads=()):
        cx.dma("sp", xt[:, :], row_ap, reads=list(extra_reads), writes=[xt], semb=xt)
        act(cx, xn[:, :], xt[:, :], AF.Square, [xt], [xn, ss], accum_out=ss[:, :])
        act(cx, rt[:, :], ss[:, :], AF.Sqrt, [ss], [rt], scale=1.0 / D, bias=EPS)
        cx.op("dve", lambda e: e.reciprocal(out=rstd[:, :], in_=rt[:, :]), [rt], [rstd])
        cx.op("dve", lambda e: e.scalar_tensor_tensor(out=xn[:, :], in0=xt[:, :], scalar=rstd[:, :],
                                                      in1=gbc[:, :], op0=ALU.mult, op1=ALU.mult),
              [xt, rstd, gbc], [xn])

    def nt_b(self, cx, xn, pT, ident, dst, dcol, eng="act"):
        for c in range(8):
            cx.op("pe", lambda e: e.transpose(pT[:, c * 128:(c + 1) * 128], xn[:, c * 128:(c + 1) * 128],
                                              ident[:, :]), [xn, ident], [pT])
        if eng == "act":
            cx.op("act", lambda e: e.copy(out=dst[:, :, dcol:dcol + 128],
                                          in_=pT[:, :].rearrange("p (c t) -> p c t", c=8)), [pT], [dst])
        else:
            cx.op("dve", lambda e: e.tensor_copy(out=dst[:, :, dcol:dcol + 128],
                                                 in_=pT[:, :].rearrange("p (c t) -> p c t", c=8)), [pT], [dst])

    def norm_transpose(self, cx, row_ap, xt, ss, rt, rstd, xn, pT, gbc, ident, dst, dcol):
        self.nt_a(cx, row_ap, xt, ss, rt, rstd, xn, gbc)
        self.nt_b(cx, xn, pT, ident, dst, dcol)

    def phase_proj(self, L, kv_row, xown):
        cx, nc, d = self.cx, self.nc, self.dr
        sfx = f"_{L}"
        S, S2 = self.S, self.S2
        with ExitStack() as ph:
            wt = cx.sb(ph, [128, 8, 3072], BF16, "wt")
            wv = d["w_in" + sfx].rearrange("(c p) n -> p c n", p=128)
            wtg = [T(wt.t) for _ in range(6)]
            order = [1, 4, 2, 5, 0, 3] if self.first_layer else [0, 3, 1, 4, 2, 5]
            for g_ in order:
                for j in (2 * g_, 2 * g_ + 1):
                    cx.dma("pool", wt[:, :, j * 256:(j + 1) * 256], wv[:, :, j * 256:(j + 1) * 256],
                           writes=[wtg[g_]], semb=wtg[g_])
            gbc = cx.sb(ph, [128, D], F32, "gbc")
            cx.dma("sp", gbc[:, :], d["gmix" + sfx][:, :], writes=[gbc], semb=gbc)
            ident = cx.sb(ph, [128, 128], BF16, "ident")
            cx.dma("pool", ident[:, :], d["ident"][:, :], writes=[ident], semb=ident)
            bd = cx.sb(ph, [128, 128], BF16, "bd")
            cx.dma("pool", bd[:, :], d["bd"][:, :], writes=[bd], semb=bd)
            gq = cx.sb(ph, [128, 1], F32, "gq")
            gk = cx.sb(ph, [128, 1], F32, "gk")
            cx.dma("sp", gq[:, :], d["gq" + sfx][:, :], writes=[gq], semb=gq)
            cx.dma("sp", gk[:, :], d["gk" + sfx][:, :], writes=[gk], semb=gk)
            gq8 = cx.sb(ph, [128, 1], F32, "gq8")
            cx.op("dve", lambda e: e.tensor_scalar(out=gq8[:, :], in0=gq[:, :], scalar1=0.125, scalar2=None,
                                                   op0=ALU.mult), [gq], [gq8])
            xts = [cx.sb(ph, [128, D], F32, f"xt{i}") for i in range(4)]
            xns = [cx.sb(ph, [128, D], BF16, f"xn{i}") for i in range(4)]
            sss = [cx.sb(ph, [128, 1], F32, f"ss{i}") for i in range(4)]
            rts = [cx.sb(ph, [128, 1], F32, f"rt{i}") for i in range(4)]
            rss = [cx.sb(ph, [128, 1], F32, f"rs{i}") for i in range(4)]
            pTs = [cx.ps(ph, [128, D], BF16, f"pT{i}") for i in range(2)]
            hTs = [cx.sb(ph, [128, 8, 512], BF16, f"hT{i}") for i in range(2)]
            pms = [cx.ps(ph, [128, 512], F32, f"pm{i}") for i in range(4)]
            stg = [cx.sb(ph, [128, 512], BF16, f"stg{i}") for i in range(4)]
            sqs = [cx.sb(ph, [128, 512], BF16, f"sq{i}") for i in range(2)]
            rinv = [cx.sb(ph, [128, 512], F32, f"rinv{i}") for i in range(2)]
            cnt = {"x": 0, "pm": 0, "stg": 0, "sq": 0}

            def fm_proj(hT, col0, fc, kind, dst_t, dst_ap_fn):
                if True:
                    pm = pms[cnt["pm"] % 4]
                    cnt["pm"] += 1
                    c0 = col0 + fc * 128
                    for c in range(8):
                        mm(cx, pm[:, :], wt[:, c, c0:c0 + 128], hT[:, c, :], c == 0, c == 7, [wtg[c0 // 512], hT], [pm])
                    st = stg[cnt["stg"] % 4]
                    cnt["stg"] += 1
                    if kind == "sbk":
                        cx.op("dve", lambda e: e.tensor_copy(out=st[:, :], in_=pm[:, :]), [pm], [st])
                    elif kind == "sbq":
                        act(cx, st[:, :], pm[:, :], AF.Copy, [pm], [st], scale=0.125)
                    else:
                        sq = sqs[cnt["sq"] % 2]
                        rv = rinv[cnt["sq"] % 2]
                        cnt["sq"] += 1
                        pm2 = pms[cnt["pm"] % 4]
                        cnt["pm"] += 1
                        act(cx, sq[:, :], pm[:, :], AF.Square, [pm], [sq])
                        mm(cx, pm2[:, :], bd[:, :], sq[:, :], True, True, [bd, sq], [pm2])
                        act(cx, rv[:, :], pm2[:, :], AF.Sqrt, [pm2], [rv], scale=1.0 / 64, bias=EPS)
                        cx.op("dve", lambda e: e.reciprocal(out=rv[:, :], in_=rv[:, :]), [rv], [rv])
                        gcol = gq8 if kind == "dfq" else gk
                        cx.op("dve", lambda e: e.scalar_tensor_tensor(
                            out=st[:, :], in0=pm[:, :], scalar=gcol[:, :], in1=rv[:, :],
                            op0=ALU.mult, op1=ALU.mult), [pm, gcol, rv], [st])
                    cx.dma("sp", dst_ap_fn(fc), st[:, :], reads=[st], writes=[], semb=st)

            def tm_proj(hT, u, col0, dst_t, dst_ap):
                pm = pms[cnt["pm"] % 4]
                cnt["pm"] += 1
                for c in range(8):
                    mm(cx, pm[:, :], hT[:, c, u * 128:(u + 1) * 128], wt[:, c, col0:col0 + 512],
                       c == 0, c == 7, [wtg[col0 // 512], hT], [pm])
                st = stg[cnt["stg"] % 4]
                cnt["stg"] += 1
                cx.op("dve", lambda e: e.tensor_copy(out=st[:, :], in_=pm[:, :]), [pm], [st])
                cx.dma("sp", dst_ap, st[:, :], reads=[st], writes=[], semb=st)

            sts_kv = [("kv", kv_row, i) for i in range(S // 512)]
            sts_q = [("q", (lambda g: xown[g * 128:(g + 1) * 128, :]), i) for i in range(S2 // 512)]
            sts = (sts_kv + sts_q) if self.first_layer else (sts_q + sts_kv)

            def chunks_of(n):
                kind, src, st_i = sts[n]
                hT = hTs[n % 2]
                t0 = st_i * 512
                out = []
                if kind == "kv":
                    for fc in range(4):
                        out.append(lambda fc=fc: fm_proj(hT, 512, fc, "sbk", None,
                                                         lambda f: d["KT"][f * 128:(f + 1) * 128, t0:t0 + 512]))
                    for fc in range(4):
                        out.append(lambda fc=fc: fm_proj(hT, 2048, fc, "dfk", None,
                                                         lambda f: d["KT"][512 + f * 128:512 + (f + 1) * 128, t0:t0 + 512]))
                    for u in range(4):
                        r0 = t0 + u * 128
                        out.append(lambda u=u, r0=r0: tm_proj(hT, u, 1024, None, d["V"][r0:r0 + 128, 0:512]))
                        out.append(lambda u=u, r0=r0: tm_proj(hT, u, 2560, None, d["V"][r0:r0 + 128, 512:1024]))
                else:
                    for fc in range(4):
                        out.append(lambda fc=fc: fm_proj(hT, 0, fc, "sbq", None,
                                                         lambda f: d["QT"][f * 128:(f + 1) * 128, t0:t0 + 512]))
                    for fc in range(4):
                        out.append(lambda fc=fc: fm_proj(hT, 1536, fc, "dfq", None,
                                                         lambda f: d["QT"][512 + f * 128:512 + (f + 1) * 128, t0:t0 + 512]))
                return out

            def a_part(n, u):
                kind, src, st_i = sts[n]
                xr = [self.tXF] if (kind == "kv" and not self.first_layer) else []
                self.nt_a(cx, src(st_i * 4 + u), xts[u], sss[u], rts[u], rss[u], xns[u], gbc, extra_reads=xr)

            def b_part(n, u):
                self.nt_b(cx, xns[u], pTs[u % 2], ident, hTs[n % 2], u * 128)

            for u in range(4):
                a_part(0, u)
            for u in range(4):
                b_part(0, u)
            for n in range(len(sts)):
                ch = chunks_of(n)
                per = len(ch) // 4
                nxt = n + 1 < len(sts)
                for i, fn in enumerate(ch):
                    if nxt and i < 4:
                        a_part(n + 1, i)
                    fn()
                    if nxt and (i + 1) % per == 0 and False:
                        pass
                if nxt:
                    for u in range(4):
                        b_part(n + 1, u)
            cx.barrier()

    def tiles(self):
        out = []
        for q in range(self.NQ):
            nkb = 8 * q + 8
            for u in range(nkb):
                kb = nkb - 1 - u
                r = kb - 8 * q
                i0 = (r // 2) if r >= 0 else 0
                out.append(dict(q=q, u=u, kb=kb, r=r, i0=i0, t0=128 * i0, first=(u == 0), last=(u == nkb - 1)))
        return out

    def phase_attn(self, L):
        cx, nc, d = self.cx, self.nc, self.dr
        sfx = f"_{L}"
        S, S2, NB = self.S, self.S2, self.NB
        lam_init = 0.8 - 0.6 * math.exp(-0.3 * L)
        with ExitStack() as ph0:
            ysb = cx.sb(ph0, [128, self.NSLOT, 512], BF16, "ysb")
            ydf = cx.sb(ph0, [128, self.NSLOT, 512], BF16, "ydf")
            self.ysb, self.ydf = ysb, ydf
            tl = self.tiles()
            with ExitStack() as ph:
                maskS = cx.sb(ph, [128, 8, 512], BF16, "maskS")
                cx.dma("pool", maskS[:, :, :], d["maskS"][:, :, :], writes=[maskS], semb=maskS)
                cx.op("dve", lambda e: e.tensor_scalar(out=maskS[:, :, :], in0=maskS[:, :, :], scalar1=-1.0,
                                                       scalar2=30000.0, op0=ALU.add, op1=ALU.mult), [maskS], [maskS])
                identA = cx.sb(ph, [128, 128], BF16, "identA")
                cx.dma("pool", identA[:, :], d["ident"][:, :], writes=[identA], semb=identA)
                negU = cx.sb(ph, [128, 128], BF16, "negU")
                cx.dma("pool", negU[:, :], d["negU"][:, :], writes=[negU], semb=negU)
                ones = cx.sb(ph, [128, 1], BF16, "ones")
                cx.op("dve", lambda e: e.memset(ones[:, :], 1.0), [], [ones])
                kts = [cx.sb(ph, [128, S], BF16, f"kt{i}") for i in range(2)]
                qts = [cx.sb(ph, [128, S2], BF16, f"qt{i}") for i in range(2)]
                vvs = [cx.sb(ph, [128, NB, 128], BF16, f"vv{i}") for i in range(2)]
                psA = [cx.ps(ph, [128, 2, 512], F32, f"psA{i}") for i in range(3)]
                psO = cx.ps(ph, [128, 8, 64], F32, "psO")
                psC = cx.ps(ph, [128, 512], F32, "psC")
                eb = [cx.sb(ph, [128, 2, 512], F32, f"eb{i}") for i in range(2)]
                spb = [cx.sb(ph, [128, 2, 512], BF16, f"spb{i}") for i in range(2)]
                wtb = [cx.sb(ph, [128, 2, 512], BF16, f"wtb{i}") for i in range(2)]
                fb = [cx.sb(ph, [128, 2, 4], F32, f"fb{i}") for i in range(2)]
                oacc = [cx.sb(ph, [128, 64], F32, f"oacc{i}") for i in range(4)]
                cbuf = [cx.sb(ph, [128, 2, 4], F32, f"carry{i}") for i in range(3)]
                pairs = [(tl[2 * m], tl[2 * m + 1]) for m in range(len(tl) // 2)]
                n = len(pairs)

                def load_hp(hp_):
                    kt_, qt_, vv_ = kts[hp_ % 2], qts[hp_ % 2], vvs[hp_ % 2]
                    cx.dma("sp", kt_[:, :], d["KT"][hp_ * 128:(hp_ + 1) * 128, :], reads=[], writes=[kt_], semb=kt_)
                    cx.dma("sp", qt_[:, :], d["QT"][hp_ * 128:(hp_ + 1) * 128, :], reads=[], writes=[qt_], semb=qt_)
                    cx.dma("sp", vv_[:, :, :],
                           d["V"][:, hp_ * 128:(hp_ + 1) * 128].rearrange("(b p) c -> p b c", p=128),
                           reads=[], writes=[vv_], semb=vv_)

                load_hp(0)
                for hp in range(4):
                    kt, qt, vv = kts[hp % 2], qts[hp % 2], vvs[hp % 2]
                    if hp + 1 < 4:
                        load_hp(hp + 1)
                    for hh in range(2):
                        h = 2 * hp + hh
                        rows = slice(hh * 64, hh * 64 + 64)

                        def s1(m):
                            A = psA[m % 3]
                            for x, t in enumerate(pairs[m]):
                                t0, kb = t["t0"], t["kb"]
                                mm(cx, A[:, x, t0:512], kt[rows, kb * 128:(kb + 1) * 128],
                                   qt[rows, t["q"] * 512 + t0:(t["q"] + 1) * 512], True, False, [kt, qt], [A],
                                   skip_group_check=True)
                                if t["r"] >= 0:
                                    mm(cx, A[:, x, t0:512], identA[:, :], maskS[:, t["r"], t0:512], False, False,
                                       [identA, maskS], [A], skip_group_check=True)

                        def sE(m):
                            t0 = pairs[m][0]["t0"]
                            A, e_ = psA[m % 3], eb[m % 2]
                            act(cx, e_[:, :, t0:512], A[:, :, t0:512], AF.Exp, [A], [e_])

                        def sSP(m):
                            t0 = pairs[m][0]["t0"]
                            e_, sp = eb[m % 2], spb[m % 2]
                            act(cx, sp[:, :, t0:512], e_[:, :, t0:512], AF.Ln, [e_], [sp], bias=1.0)

                        def sBC(m):
                            B, sp, C = psA[m % 3], spb[m % 2], psC
                            t0, i0 = pairs[m][0]["t0"], pairs[m][0]["i0"]
                            for x, t in enumerate(pairs[m]):
                                mm(cx, B[:, x, t0:512], negU[:, :], sp[:, x, t0:512], False, True, [negU, sp], [B],
                                   skip_group_check=True)
                            cc, cn = cbuf[m % 3], cbuf[(m + 1) % 3]
                            if pairs[m][0]["first"]:
                                cx.op("dve", lambda e: e.memset(cc[:, :, :], 0.0), [], [cc])
                            elif i0 > 0:
                                cx.op("dve", lambda e: e.memset(cc[:, 1, 0:i0], 0.0), [], [cc])
                            for x, t in enumerate(pairs[m]):
                                if t["last"]:
                                    continue
                                for i in range(i0, 4):
                                    mm(cx, C[:, 4 * x + i:4 * x + i + 1], sp[:, x, i * 128:(i + 1) * 128], ones[:, :],
                                       True, True, [sp, ones], [C])
                            cx.op("dve", lambda e: e.tensor_tensor(out=cc[:, 1, i0:4], in0=cc[:, 0, i0:4], in1=C[:, i0:4],
                                                                   op=ALU.add), [cc, C], [cc])
                            if not pairs[m][1]["last"]:
                                cx.op("dve", lambda e: e.memset(cn[:, 0, :], 0.0), [], [cn])
                                cx.op("dve", lambda e: e.tensor_tensor(out=cn[:, 0, i0:4], in0=cc[:, 1, i0:4],
                                                                       in1=C[:, 4 + i0:8], op=ALU.add), [cc, C], [cn])

                        def sW(m):
                            t0 = pairs[m][0]["t0"]
                            B, w_, f_ = psA[m % 3], wtb[m % 2], fb[m % 2]
                            act(cx, w_[:, :, t0:512], B[:, :, t0:512], AF.Exp, [B], [w_])
                            act(cx, f_[:, :, :], cbuf[m % 3][:, :, :], AF.Exp, [cbuf[m % 3]], [f_], scale=-1.0)

                        def sPV(m):
                            w_, O, f_ = wtb[m % 2], psO, fb[m % 2]
                            i0 = pairs[m][0]["i0"]
                            for x, t in enumerate(pairs[m]):
                                kb = t["kb"]
                                for i in range(i0, 4):
                                    mm(cx, O[:, 4 * x + i, :], w_[:, x, i * 128:(i + 1) * 128],
                                       vv[:, kb, hh * 64:hh * 64 + 64], True, True, [w_, vv], [O])
                            for x, t in enumerate(pairs[m]):
                                q = t["q"]
                                if t["first"]:
                                    for i in range(4):
                                        if i >= i0:
                                            cx.op("dve", lambda e: e.tensor_copy(out=oacc[i][:, :], in_=O[:, 4 * x + i, :]),
                                                  [O], [oacc[i]])
                                        else:
                                            cx.op("dve", lambda e: e.memset(oacc[i][:, :], 0.0), [], [oacc[i]])
                                else:
                                    for i in range(i0, 4):
                                        dst = ysb[:, 4 * q + i, h * 64:(h + 1) * 64] if t["last"] else oacc[i][:, :]
                                        wr = [ysb] if t["last"] else [oacc[i]]
                                        cx.op("dve", lambda e: e.scalar_tensor_tensor(
                                            out=dst, in0=O[:, 4 * x + i, :], scalar=f_[:, x, i:i + 1], in1=oacc[i][:, :],
                                            op0=ALU.mult, op1=ALU.add), [O, f_, oacc[i]], wr)

                        ok = lambda k: 0 <= k < n
                        for j in range(-2, n + 2):
                            if ok(j):
                                sBC(j)
                            if ok(j + 1):
                                sE(j + 1)
                            if ok(j - 1):
                                sW(j - 1)
                            if ok(j + 2):
                                s1(j + 2)
                            if ok(j + 1):
                                sSP(j + 1)
                            if ok(j - 2):
                                sPV(j - 2)
                cx.barrier()
            with ExitStack() as ph:
                maskD = cx.sb(ph, [128, 8, 512], BF16, "maskD")
                cx.dma("pool", maskD[:, :, :], d["maskD"][:, :, :], writes=[maskD], semb=maskD)
                cx.op("dve", lambda e: e.tensor_scalar(out=maskD[:, :, :], in0=maskD[:, :, :], scalar1=-1.0,
                                                       scalar2=30000.0, op0=ALU.add, op1=ALU.mult), [maskD], [maskD])
                identD = cx.sb(ph, [128, 128], BF16, "identD")
                cx.dma("pool", identD[:, :], d["ident"][:, :], writes=[identD], semb=identD)
                gsub = cx.sb(ph, [128, 128], F32, "gsub")
                cx.dma("sp", gsub[:, :], d["gsub" + sfx][:, :], writes=[gsub], semb=gsub)
                cx.op("dve", lambda e: e.tensor_scalar(out=gsub[:, :], in0=gsub[:, :], scalar1=(1.0 - lam_init),
                                                       scalar2=None, op0=ALU.mult), [gsub], [gsub])
                row = cx.sb(ph, [1, 128], F32, "gqkrow")
                cx.dma("sp", row[:, :], d["gqk_row" + sfx][:, :], writes=[row], semb=row)
                lrow = cx.sb(ph, [1, 256], F32, "lamrow")
                cx.dma("sp", lrow[:, :], d["lam_row" + sfx][:, :], writes=[lrow], semb=lrow)
                sc = cx.sb(ph, [1, 8], F32, "sc")
                prod = cx.sb(ph, [1, 128], F32, "prod")
                ones1 = cx.sb(ph, [1, 128], F32, "ones1")
                cx.op("dve", lambda e: e.memset(ones1[:, :], 1.0), [], [ones1])
                cx.op("dve", lambda e: e.memset(sc[:, :], 0.0), [], [sc])
                cx.op("dve", lambda e: e.reduce_max(out=sc[:, 0:1], in_=row[:, 0:64], axis=AX.X, apply_absolute_value=True), [row], [sc])
                cx.op("dve", lambda e: e.reduce_max(out=sc[:, 1:2], in_=row[:, 64:128], axis=AX.X, apply_absolute_value=True), [row], [sc])
                cx.op("dve", lambda e: e.scalar_tensor_tensor(out=sc[:, 2:3], in0=sc[:, 0:1], scalar=-8.0,
                                                              in1=sc[:, 1:2], op0=ALU.mult, op1=ALU.mult),
                      [sc], [sc])
                cx.op("dve", lambda e: e.tensor_tensor(out=prod[:, 0:64], in0=lrow[:, 0:64], in1=lrow[:, 64:128],
                                                       op=ALU.mult), [lrow], [prod])
                cx.op("dve", lambda e: e.tensor_tensor(out=prod[:, 64:128], in0=lrow[:, 128:192],
                                                       in1=lrow[:, 192:256], op=ALU.mult), [lrow], [prod])
                cx.op("dve", lambda e: e.reduce_sum(out=sc[:, 4:5], in_=prod[:, 0:64], axis=AX.X), [prod], [sc])
                cx.op("dve", lambda e: e.reduce_sum(out=sc[:, 5:6], in_=prod[:, 64:128], axis=AX.X), [prod], [sc])
                act(cx, sc[:, 4:6], sc[:, 4:6], AF.Exp, [sc], [sc])
                cx.op("dve", lambda e: e.scalar_tensor_tensor(out=sc[:, 3:4], in0=sc[:, 4:5], scalar=lam_init,
                                                              in1=sc[:, 5:6], op0=ALU.add, op1=ALU.subtract),
                      [sc], [sc])
                psZ = [[cx.ps(ph, [128, 512], F32, f"psZ{i}{c}") for c in range(2)] for i in range(2)]
                psS = psZ[0][0]
                mm(cx, psS[:, 0:2], ones1[:, :], sc[:, 2:4], True, True, [ones1, sc], [psS])
                negM = cx.sb(ph, [128, 1], F32, "negM")
                lamb = cx.sb(ph, [128, 1], F32, "lamb")
                cx.op("dve", lambda e: e.tensor_copy(out=negM[:, :], in_=psS[:, 0:1]), [psS], [negM])
                cx.op("dve", lambda e: e.tensor_copy(out=lamb[:, :], in_=psS[:, 1:2]), [psS], [lamb])

                ktc = [cx.sb(ph, [68, S], BF16, f"ktd{c}") for c in range(2)]
                qtc = [cx.sb(ph, [68, S2], BF16, f"qtd{c}") for c in range(2)]
                for c in range(2):
                    cx.op("dve", lambda e: e.memset(ktc[c][64:68, :], 1.0), [], [ktc[c]])
                    cx.op("dve", lambda e: e.memset(qtc[c][64:68, :], 1.0), [], [qtc[c]])
                va = cx.sb(ph, [128, NB, 129], BF16, "va")
                cx.op("dve", lambda e: e.memset(va[:, :, 128:129], 1.0), [], [va])
                psO = [cx.ps(ph, [128, 512], F32, f"psOd{i}") for i in range(4)]
                pb = [[cx.sb(ph, [128, 512], BF16, f"pb{i}{c}") for c in range(2)] for i in range(2)]
                rl = cx.sb(ph, [128, 4], F32, "rl")
                t1 = cx.sb(ph, [128, 128], F32, "t1")
                ob = cx.sb(ph, [128, 128], F32, "ob")
                junk = cx.sb(ph, [128, 128], F32, "junk")
                ssd = cx.sb(ph, [128, 2], F32, "ssd")

                for h in range(4):
                    r0 = 512 + h * 128
                    for c in range(2):
                        cx.dma("sp", ktc[c][0:64, :], d["KT"][r0 + c * 64:r0 + c * 64 + 64, :], writes=[ktc[c]],
                               semb=ktc[c])
                        cx.dma("pool", ktc[c][64:66, :], d["kx"][h, :, :], writes=[ktc[c]], semb=ktc[c])
                        cx.dma("sp", qtc[c][0:64, :], d["QT"][r0 + c * 64:r0 + c * 64 + 64, :], writes=[qtc[c]],
                               semb=qtc[c])
                        cx.dma("pool", qtc[c][66:68, :], d["qx"][h, :, :], writes=[qtc[c]], semb=qtc[c])
                    cx.dma("sp", va[:, :, 0:128],
                           d["V"][:, r0:r0 + 128].rearrange("(b p) c -> p b c", p=128),
                           reads=[], writes=[va], semb=va)
                    n = len(tl)

                    def s1(k):
                        t = tl[k]
                        t0, kb, q = t["t0"], t["kb"], t["q"]
                        for c in range(2):
                            Z = psZ[k % 2][c]
                            mm(cx, Z[:, t0:512], ktc[c][:, kb * 128:(kb + 1) * 128],
                               qtc[c][:, q * 512 + t0:(q + 1) * 512], True, t["r"] < 0, [ktc[c], qtc[c]], [Z])
                            if t["r"] >= 0:
                                mm(cx, Z[:, t0:512], identD[:, :], maskD[:, t["r"], t0:512], False, True,
                                   [identD, maskD], [Z])

                    def sX(k):
                        t = tl[k]
                        t0 = t["t0"]
                        for c in range(2):
                            Z, p_ = psZ[k % 2][c], pb[k % 2][c]
                            act(cx, p_[:, t0:512], Z[:, t0:512], AF.Exp, [Z, negM], [p_], bias=negM[:, :])

                    def sPV(k):
                        t = tl[k]
                        t0, i0, kb, q = t["t0"], t["i0"], t["kb"], t["q"]
                        for i in range(i0, 4):
                            for c in range(2):
                                p_ = pb[k % 2][c]
                                first_i = (t["u"] == 2 * (3 - i))
                                mm(cx, psO[i][:, c * 256:c * 256 + 129], p_[:, i * 128:(i + 1) * 128],
                                   va[:, kb, :], bool(first_i and c == 0), bool(t["last"]), [p_, va], [psO[i]],
                                   skip_group_check=True)
                        if t["last"]:
                            for i in range(4):
                                O = psO[i]
                                cx.op("dve", lambda e: e.reciprocal(out=rl[:, 0:1], in_=O[:, 128:129]), [O], [rl])
                                cx.op("dve", lambda e: e.reciprocal(out=rl[:, 1:2], in_=O[:, 384:385]), [O], [rl])
                                cx.op("dve", lambda e: e.tensor_tensor(out=rl[:, 2:3], in0=rl[:, 1:2],
                                                                       in1=lamb[:, :], op=ALU.mult), [rl, lamb], [rl])
                                cx.op("dve", lambda e: e.tensor_scalar(out=t1[:, :], in0=O[:, 256:384],
                                                                       scalar1=rl[:, 2:3], scalar2=None,
                                                                       op0=ALU.mult), [O, rl], [t1])
                                cx.op("dve", lambda e: e.scalar_tensor_tensor(
                                    out=ob[:, :], in0=O[:, 0:128], scalar=rl[:, 0:1], in1=t1[:, :],
                                    op0=ALU.mult, op1=ALU.subtract), [O, rl, t1], [ob])
                                cx.op("dve", lambda e: e.tensor_tensor(out=junk[:, :], in0=ob[:, :], in1=ob[:, :],
                                                                       op=ALU.mult), [ob], [junk])
                                cx.op("dve", lambda e: e.reduce_sum(out=ssd[:, 0:1], in_=junk[:, :], axis=AX.X),
                                      [junk], [ssd])
                                act(cx, ssd[:, 1:2], ssd[:, 0:1], AF.Ln, [ssd], [ssd], scale=1.0 / 128, bias=EPS)
                                act(cx, ssd[:, 1:2], ssd[:, 1:2], AF.Exp, [ssd], [ssd], scale=-0.5)
                                cx.op("dve", lambda e: e.scalar_tensor_tensor(
                                    out=ydf[:, 4 * q + i, h * 128:(h + 1) * 128], in0=ob[:, :], scalar=ssd[:, 1:2],
                                    in1=gsub[:, :], op0=ALU.mult, op1=ALU.mult), [ob, ssd, gsub], [ydf])

                    ok = lambda k: 0 <= k < n
                    for j in range(-1, n + 1):
                        if ok(j + 1):
                            s1(j + 1)
                        if ok(j):
                            sX(j)
                        if ok(j - 1):
                            sPV(j - 1)
                cx.barrier()
            if self.debug:
                cx.dma("pool", d["dbg_ysb"].rearrange("(j p) c -> p j c", p=128), ysb[:, :, :], reads=[ysb],
                       writes=[], semb=ysb)
                cx.dma("pool", d["dbg_ydf"].rearrange("(j p) c -> p j c", p=128), ydf[:, :, :], reads=[ydf],
                       writes=[], semb=ydf)
            self.phase_post(L, ph0)

    def phase_post(self, L, ph0):
        cx, nc, d = self.cx, self.nc, self.dr
        sfx = f"_{L}"
        ysb, ydf = self.ysb, self.ydf
        moe = (L % 2 == 1)
        with ExitStack() as ph:
            def loadw(name, K, N, tag):
                kc = K // 128
                w = cx.sb(ph, [128, kc, N], BF16, tag)
                v = d[name + sfx].rearrange("(c p) n -> p c n", p=128)
                for j in range(0, N, 256):
                    cx.dma("pool", w[:, :, j:j + 256], v[:, :, j:j + 256], writes=[w], semb=w)
                return w
            wg = loadw("w_gate", D, 2048, "wg")
            wsb = loadw("w_bsb", 512, D, "wsb")
            wdf = loadw("w_bdf", 512, D, "wdf")
            wo = loadw("w_out", D, D, "wo")
            bg = cx.sb(ph, [1, 2048], BF16, "bg")
            cx.dma("pool", bg[:, :], d["b_gate" + sfx][:, :], writes=[bg], semb=bg)
            onesr = cx.sb(ph, [1, 128], BF16, "onesr")
            cx.op("dve", lambda e: e.memset(onesr[:, :], 1.0), [], [onesr])
            gbc = cx.sb(ph, [128, D], F32, "gbc1")
            cx.dma("sp", gbc[:, :], d["gmix" + sfx][:, :], writes=[gbc], semb=gbc)
            gbc2 = cx.sb(ph, [128, D], F32, "gbc2")
            cx.dma("sp", gbc2[:, :], d["gffn" + sfx][:, :], writes=[gbc2], semb=gbc2)
            ident = cx.sb(ph, [128, 128], BF16, "ident2")
            cx.dma("pool", ident[:, :], d["ident"][:, :], writes=[ident], semb=ident)
            if moe:
                zt = cx.sb(ph, [128, 2, D], BF16, "zt")
                cx.op("dve", lambda e: e.memset(zt[:, :, :], 0.0), [], [zt])
                for r0_ in range(0, self.NBLK * 512, 256):
                    cx.dma("pool", d["XS"][r0_:r0_ + 256, :].rearrange("(u p) c -> p u c", p=128), zt[:, :, :],
                           reads=[zt], writes=[], semb=zt)
                wr = loadw_r = cx.sb(ph, [128, 8, N_EXP], BF16, "wr")
                cx.dma("pool", wr[:, :, :], d["w_r" + sfx].rearrange("(c p) n -> p c n", p=128), writes=[wr], semb=wr)
            xts = [cx.sb(ph, [128, D], F32, f"pxt{i}") for i in range(2)]
            xn = cx.sb(ph, [128, D], BF16, "pxn")
            ss = cx.sb(ph, [128, 1], F32, "pss")
            rt = cx.sb(ph, [128, 1], F32, "prt")
            rs = cx.sb(ph, [128, 1], F32, "prs")
            pT = cx.ps(ph, [128, D], BF16, "ppT")
            hT = cx.sb(ph, [128, 8, 128], BF16, "phT")
            yT = cx.sb(ph, [128, 8, 128], BF16, "pyT")
            mT = cx.sb(ph, [128, 8, 128], BF16, "pmT")
            h2T = cx.sb(ph, [128, 8, 128], BF16, "ph2T")
            gates = cx.sb(ph, [128, 2048], F32, "gates")
            mg = cx.sb(ph, [128, D], F32, "mg")
            mgb = cx.sb(ph, [128, D], BF16, "mgb")
            x1 = [cx.sb(ph, [128, D], F32, f"x1{i}") for i in range(2)]
            pm = [cx.ps(ph, [128, 512], F32, f"ppm{i}") for i in range(4)]
            pr = cx.ps(ph, [128, N_EXP], F32, "ppr")
            lg = cx.sb(ph, [128, N_EXP], F32, "lg")
            l2 = cx.sb(ph, [128, N_EXP], F32, "l2")
            mk = cx.sb(ph, [128, N_EXP], F32, "mk")
            ex = cx.sb(ph, [128, N_EXP], F32, "ex")
            sm = cx.sb(ph, [128, 4], F32, "sm")
            npm = 0
            xnF = [cx.sb(ph, [128, D], BF16, f"pxnF{i}") for i in range(2)]
            hTs_ = [cx.sb(ph, [128, 8, 128], BF16, f"phT{i}") for i in range(2)]
            yTs_ = [cx.sb(ph, [128, 8, 128], BF16, f"pyT{i}") for i in range(2)]
            ssF = [cx.sb(ph, [128, 1], F32, f"pssF{i}") for i in range(2)]
            rtF = [cx.sb(ph, [128, 1], F32, f"prtF{i}") for i in range(2)]
            rsF = [cx.sb(ph, [128, 1], F32, f"prsF{i}") for i in range(2)]
            pTa = cx.ps(ph, [128, D], BF16, "ppTa")

            def front(j):
                k = j % 2
                self.nt_a(cx, self.cur_xown[j * 128:(j + 1) * 128, :], xts[k], ssF[k], rtF[k], rsF[k], xnF[k], gbc)
                self.nt_b(cx, xnF[k], pTa, ident, hTs_[k], 0)
                for c in range(4):
                    cx.op("pe", lambda e: e.transpose(pTa[:, c * 128:(c + 1) * 128], ysb[:, j, c * 128:(c + 1) * 128],
                                                      ident[:, :]), [ysb, ident], [pTa])
                for c in range(4):
                    cx.op("pe", lambda e: e.transpose(pTa[:, (4 + c) * 128:(5 + c) * 128],
                                                      ydf[:, j, c * 128:(c + 1) * 128], ident[:, :]),
                          [ydf, ident], [pTa])
                cx.op("dve", lambda e: e.tensor_copy(out=yTs_[k][:, :, :],
                                                     in_=pTa[:, :].rearrange("p (c t) -> p c t", c=8)), [pTa], [yTs_[k]])

            front(0)
            for j in range(self.NSLOT):
                xt = xts[j % 2]
                xo = x1[j % 2]
                hT = hTs_[j % 2]
                yT = yTs_[j % 2]
                if j + 1 < self.NSLOT:
                    front(j + 1)
                for gi in range(4):
                    P = pm[npm % 4]
                    npm += 1
                    for c in range(8):
                        mm(cx, P[:, :], hT[:, c, :], wg[:, c, gi * 512:(gi + 1) * 512], c == 0, False, [hT, wg], [P])
                    mm(cx, P[:, :], onesr[:, :], bg[:, gi * 512:(gi + 1) * 512], False, True, [onesr, bg], [P])
                    act(cx, gates[:, gi * 512:(gi + 1) * 512], P[:, :], AF.Sigmoid, [P], [gates])
                for nh in range(2):
                    Pa = pm[npm % 4]
                    npm += 1
                    Pb = pm[npm % 4]
                    npm += 1
                    for c in range(4):
                        mm(cx, Pa[:, :], yT[:, c, :], wsb[:, c, nh * 512:(nh + 1) * 512], c == 0, c == 3, [yT, wsb], [Pa])
                    for c in range(4):
                        mm(cx, Pb[:, :], yT[:, 4 + c, :], wdf[:, c, nh * 512:(nh + 1) * 512], c == 0, c == 3,
                           [yT, wdf], [Pb])
                    sl = slice(nh * 512, (nh + 1) * 512)
                    sl2 = slice(1024 + nh * 512, 1024 + (nh + 1) * 512)
                    cx.op("dve", lambda e: e.tensor_tensor(out=mg[:, sl], in0=Pa[:, :], in1=gates[:, sl], op=ALU.mult),
                          [Pa, gates], [mg])
                    cx.op("dve", lambda e: e.tensor_tensor(out=gates[:, sl2], in0=Pb[:, :], in1=gates[:, sl2],
                                                           op=ALU.mult), [Pb, gates], [gates])
                    cx.op("dve", lambda e: e.tensor_tensor(out=mgb[:, sl], in0=mg[:, sl], in1=gates[:, sl2],
                                                           op=ALU.add), [mg, gates], [mgb])
                for c in range(8):
                    cx.op("pe", lambda e: e.transpose(pT[:, c * 128:(c + 1) * 128], mgb[:, c * 128:(c + 1) * 128],
                                                      ident[:, :]), [mgb, ident], [pT])
                cx.op("act", lambda e: e.copy(out=mT[:, :, :], in_=pT[:, :].rearrange("p (c t) -> p c t", c=8)),
                      [pT], [mT])
                for nh in range(2):
                    P = pm[npm % 4]
                    npm += 1
                    for c in range(8):
                        mm(cx, P[:, :], mT[:, c, :], wo[:, c, nh * 512:(nh + 1) * 512], c == 0, c == 7, [mT, wo], [P])
                    sl = slice(nh * 512, (nh + 1) * 512)
                    cx.op("dve", lambda e: e.tensor_tensor(out=xo[:, sl], in0=xt[:, sl], in1=P[:, :], op=ALU.add),
                          [xt, P], [xo])
                cx.dma("sp", d["X1"][j * 128:(j + 1) * 128, :], xo[:, :], reads=[xo], writes=[], semb=xo)
                if self.debug:
                    cx.dma("sp", d["dbg_x1"][j * 128:(j + 1) * 128, :], xo[:, :], reads=[xo], writes=[],
                           semb=xo)
                act(cx, xn[:, :], xo[:, :], AF.Square, [xo], [xn, ss], accum_out=ss[:, :])
                act(cx, rt[:, :], ss[:, :], AF.Sqrt, [ss], [rt], scale=1.0 / D, bias=EPS)
                cx.op("dve", lambda e: e.reciprocal(out=rs[:, :], in_=rt[:, :]), [rt], [rs])
                cx.op("dve", lambda e: e.scalar_tensor_tensor(out=xn[:, :], in0=xo[:, :], scalar=rs[:, :],
                                                              in1=gbc2[:, :], op0=ALU.mult, op1=ALU.mult),
                      [xo, rs, gbc2], [xn])
                for c in range(8):
                    cx.op("pe", lambda e: e.transpose(pT[:, c * 128:(c + 1) * 128], xn[:, c * 128:(c + 1) * 128],
                                                      ident[:, :]), [xn, ident], [pT])
                cx.op("act", lambda e: e.copy(out=h2T[:, :, :], in_=pT[:, :].rearrange("p (c t) -> p c t", c=8)),
                      [pT], [h2T])
                cx.dma("sp", d["H2T"][:, j * 128:(j + 1) * 128].rearrange("(c p) t -> p c t", p=128), h2T[:, :, :],
                       reads=[h2T], writes=[], semb=h2T)
                if moe:
                    rw = self.rw
                    for c in range(8):
                        mm(cx, pr[:, :], h2T[:, c, :], wr[:, c, :], c == 0, c == 7, [h2T, wr], [pr])
                    cx.op("dve", lambda e: e.tensor_copy(out=lg[:, :], in_=pr[:, :]), [pr], [lg])
                    cx.op("dve", lambda e: e.reduce_max(out=sm[:, 0:1], in_=lg[:, :], axis=AX.X), [lg], [sm])
                    cx.dma("sp", d["H2"][j * 128:(j + 1) * 128, :], xn[:, :], reads=[xn], writes=[], semb=xn)
                    oh1A, selA = self.oh1A, self.selA
                    cx.op("dve", lambda e: e.tensor_scalar(out=oh1A[:, j, :], in0=lg[:, :], scalar1=sm[:, 0:1],
                                                           scalar2=None, op0=ALU.is_equal), [lg, sm], [oh1A])
                    cx.op("dve", lambda e: e.scalar_tensor_tensor(out=l2[:, :], in0=oh1A[:, j, :], scalar=NEG_BIG,
                                                                  in1=lg[:, :], op0=ALU.mult, op1=ALU.add),
                          [oh1A, lg], [l2])
                    cx.op("dve", lambda e: e.reduce_max(out=sm[:, 1:2], in_=l2[:, :], axis=AX.X), [l2], [sm])
                    cx.op("dve", lambda e: e.tensor_scalar(out=mk[:, :], in0=lg[:, :], scalar1=sm[:, 1:2],
                                                           scalar2=None, op0=ALU.is_ge), [lg, sm], [mk])
                    cx.op("dve", lambda e: e.tensor_copy(out=selA[:, j, :], in_=mk[:, :]), [mk], [selA])
                    cx.op("dve", lambda e: e.tensor_scalar(out=sm[:, 2:3], in0=sm[:, 0:1], scalar1=-1.0,
                                                           scalar2=None, op0=ALU.mult), [sm], [sm])
                    act(cx, ex[:, :], lg[:, :], AF.Exp, [lg, sm], [ex], bias=sm[:, 2:3])
                    cx.op("dve", lambda e: e.tensor_tensor(out=ex[:, :], in0=ex[:, :], in1=mk[:, :], op=ALU.mult),
                          [ex, mk], [ex])
                    cx.op("dve", lambda e: e.reduce_sum(out=sm[:, 3:4], in_=ex[:, :], axis=AX.X), [ex], [sm])
                    cx.op("dve", lambda e: e.reciprocal(out=sm[:, 3:4], in_=sm[:, 3:4]), [sm], [sm])
                    cx.op("dve", lambda e: e.tensor_scalar(out=rw[:, j, :], in0=ex[:, :], scalar1=sm[:, 3:4],
                                                           scalar2=None, op0=ALU.mult), [ex, sm], [rw])
            cx.barrier()

    def phase_ffn(self, L, last):
        cx, nc, d = self.cx, self.nc, self.dr
        sfx = f"_{L}"
        moe = (L % 2 == 1)
        FS = 512
        passes = []
        if not moe:
            f0 = 0
            while f0 < DFF:
                nf = min(FS, DFF - f0)
                passes.append((d["w_gu" + sfx][:, f0:f0 + nf], d["w_gu" + sfx][:, DFF + f0:DFF + f0 + nf],
                               d["w_dn" + sfx][f0:f0 + nf, :], None, nf))
                f0 += nf
        else:
            for e_ in range(N_EXP):
                for f0 in range(0, DFE, FS):
                    nf = min(FS, DFE - f0)
                    passes.append((d["w1" + sfx][e_, :, f0:f0 + nf], d["w3" + sfx][e_, :, f0:f0 + nf],
                                   d["w2" + sfx][e_, f0:f0 + nf, :], e_, nf))
        GT = min(16, self.NSLOT)
        ngroups = self.NSLOT // GT
        with ExitStack() as ph:
            acc = cx.sb(ph, [128, GT, D], F32, "acc")
            h2 = cx.sb(ph, [128, 8, GT * 128], BF16, "h2")
            w1s = [cx.sb(ph, [128, 8, FS], BF16, f"w1s{i}") for i in range(2)]
            w3s = [cx.sb(ph, [128, 8, FS], BF16, f"w3s{i}") for i in range(2)]
            w2s = [cx.sb(ph, [128, FS // 128, D], BF16, f"w2s{i}") for i in range(2)]
            pg = [cx.ps(ph, [128, 512], F32, f"pg{i}") for i in range(2)]
            pu = [cx.ps(ph, [128, 512], F32, f"pu{i}") for i in range(2)]
            po = [cx.ps(ph, [128, 512], F32, f"po{i}") for i in range(2)]
            sg = [cx.sb(ph, [128, 512], F32, f"sg{i}") for i in range(2)]
            hid = [cx.sb(ph, [128, FS // 128, 512], BF16, f"hid{i}") for i in range(2)]
            rw = self.rw if moe else None
            npass = len(passes)
            if not last:
                xtmp = [cx.sb(ph, [128, D], F32, f"xtmp{i}") for i in range(2)]
                pmk = cx.sb(ph, [128, 2], F32, "pmk")
                cx.dma("sp", pmk[:, :], d["pmask"][:, :], writes=[pmk], semb=pmk)

            def load_pass(pi):
                w1a, w3a, w2a, _, nf = passes[pi]
                k = pi % 2
                fc = nf // 128
                cx.dma("pool", w1s[k][:, :, 0:nf], w1a.rearrange("(c p) n -> p c n", p=128), writes=[w1s[k]], semb=w1s[k])
                cx.dma("pool", w3s[k][:, :, 0:nf], w3a.rearrange("(c p) n -> p c n", p=128), writes=[w3s[k]], semb=w3s[k])
                for c in range(fc):
                    cx.dma("pool", w2s[k][:, c, :], w2a[c * 128:(c + 1) * 128, :], writes=[w2s[k]], semb=w2s[k])

            def group_inputs(g_):
                s0_ = g_ * GT
                cx.dma("sp", h2[:, :, :], d["H2T"][:, s0_ * 128:(s0_ + GT) * 128].rearrange("(c p) t -> p c t", p=128),
                       reads=[], writes=[h2], semb=h2)
                load_pass(0)
                if npass > 1:
                    load_pass(1)

            group_inputs(0)
            for g in range(ngroups):
                s0 = g * GT
                cx.dma("sp", acc[:, :, :], d["X1"][s0 * 128:(s0 + GT) * 128, :].rearrange("(j p) c -> p j c", p=128),
                       reads=[], writes=[acc], semb=acc)
                nst = GT // 4
                units = [(pi, st) for pi in range(npass) for st in range(nst)]

                def gu(n):
                    pi, st = units[n]
                    nf = passes[pi][4]
                    k = pi % 2
                    fc = nf // 128
                    hd = hid[n % 2]
                    for f in range(fc):
                        G_, U_ = pg[f % 2], pu[f % 2]
                        for c in range(8):
                            mm(cx, G_[:, :], w1s[k][:, c, f * 128:(f + 1) * 128], h2[:, c, st * 512:(st + 1) * 512],
                               c == 0, c == 7, [w1s[k], h2], [G_])
                        for c in range(8):
                            mm(cx, U_[:, :], w3s[k][:, c, f * 128:(f + 1) * 128], h2[:, c, st * 512:(st + 1) * 512],
                               c == 0, c == 7, [w3s[k], h2], [U_])
                        s_ = sg[f % 2]
                        act(cx, s_[:, :], G_[:, :], AF.Silu, [G_], [s_])
                        cx.op("dve", lambda e: e.tensor_tensor(out=hd[:, f, :], in0=s_[:, :], in1=U_[:, :],
                                                               op=ALU.mult), [s_, U_], [hd])

                def oo(n):
                    pi, st = units[n]
                    ex_, nf = passes[pi][3], passes[pi][4]
                    k = pi % 2
                    fc = nf // 128
                    hd = hid[n % 2]
                    for u in range(4):
                        j = st * 4 + u
                        for nh in range(2):
                            O = po[nh]
                            for f in range(fc):
                                mm(cx, O[:, :], hd[:, f, u * 128:(u + 1) * 128], w2s[k][:, f, nh * 512:(nh + 1) * 512],
                                   f == 0, f == fc - 1, [hd, w2s[k]], [O])
                            sl = slice(nh * 512, (nh + 1) * 512)
                            if ex_ is None:
                                cx.op("dve", lambda e: e.tensor_tensor(out=acc[:, j, sl], in0=acc[:, j, sl],
                                                                       in1=O[:, :], op=ALU.add), [acc, O], [acc])
                            else:
                                cx.op("dve", lambda e: e.scalar_tensor_tensor(
                                    out=acc[:, j, sl], in0=O[:, :], scalar=rw[:, s0 + j, ex_:ex_ + 1],
                                    in1=acc[:, j, sl], op0=ALU.mult, op1=ALU.add), [O, rw, acc], [acc])

                gu(0)
                for n in range(len(units)):
                    if n + 1 < len(units):
                        gu(n + 1)
                    oo(n)
                    pi, st = units[n]
                    if st == nst - 1 and pi + 2 < npass:
                        load_pass(pi + 2)
                if g + 1 < ngroups:
                    group_inputs(g + 1)
                dst = d["xout"] if last else d["XOWN2"]
                cx.dma("sp", dst[s0 * 128:(s0 + GT) * 128, :].rearrange("(j p) c -> p j c", p=128), acc[:, :, :],
                       reads=[acc], writes=[], semb=acc)
                if not last:
                    for j in range(GT):
                        for pp in range(2):
                            tm = xtmp[(2 * j + pp) % 2]
                            cx.op("dve", lambda e: e.tensor_scalar(out=tm[:, :], in0=acc[:, j, :], scalar1=pmk[:, pp:pp + 1],
                                                                   scalar2=None, op0=ALU.mult), [acc, pmk], [tm])
                            cx.dma("sp", d["XG2"][pp, (s0 + j) * 128:(s0 + j + 1) * 128, :], tm[:, :], reads=[tm],
                                   writes=[], semb=tm)
                    P_ = cx.E["pool"]
                    for tm in xtmp:
                        for s_ in (tm.dsem or {}).values():
                            if P_.seen.get(id(s_), 0) < s_.total:
                                P_.eng.wait_ge(s_.h, s_.total)
                                P_.seen[id(s_)] = s_.total
                    xg = d["XG2"].rearrange("a r c -> (a r) c")
                    xf = d["XF2"].rearrange("a r c -> (a r) c")
                    for pp in range(2):
                        for r0 in range(0, GT * 128, 1024):
                            rr = pp * self.S2 + s0 * 128 + r0
                            nrows = min(1024, GT * 128 - r0)
                            ins = nc.gpsimd.collective_compute(
                                "AllReduce", ALU.add,
                                replica_groups=[[2 * i, 2 * i + 1] for i in range(self.ncores // 2)],
                                ins=[xg[rr:rr + nrows, :]], outs=[xf[rr:rr + nrows, :]])
                            ins.then_inc(self.ccs.h)
                            self.ccs.total += 1
                    self.tXF.lw = (self.ccs, self.ccs.total)
            cx.barrier()


    def idma(self, out, in_, out_off, in_off, reads, writes, semb):
        cx = self.cx
        Q = cx.E["pool"]
        cx._sync(Q, reads, writes)
        ds = cx.dsem_for(semb, "sw")
        ins = Q.eng.indirect_dma_start(out=out, out_offset=out_off, in_=in_, in_offset=in_off)
        ds.total += 16
        ins.then_inc(ds.h, 16)
        cx._reg((ds, ds.total), reads, writes)

    def phase_moe_sparse(self, L):
        cx, nc, d = self.cx, self.nc, self.dr
        sfx = f"_{L}"
        NS, E, NBLK, NSL = self.NSLOT, N_EXP, self.NBLK, 7
        I32 = mybir.dt.int32
        IOA = bass.IndirectOffsetOnAxis
        selA, oh1A, rw = self.selA, self.oh1A, self.rw
        with ExitStack() as ph:
            pT = cx.ps(ph, [128, D], BF16, "mpT")
            pg = [cx.ps(ph, [128, 512], F32, f"mpg{i}") for i in range(2)]
            pu = [cx.ps(ph, [128, 512], F32, f"mpu{i}") for i in range(2)]
            po = [cx.ps(ph, [128, 512], F32, f"mpo{i}") for i in range(2)]
            ident = cx.sb(ph, [128, 128], BF16, "mident")
            cx.dma("pool", ident[:, :], d["ident"][:, :], writes=[ident], semb=ident)
            ltri = cx.sb(ph, [128, 128], BF16, "ltri")
            cx.dma("pool", ltri[:, :], d["ltri"][:, :], writes=[ltri], semb=ltri)
            onesq = cx.sb(ph, [128, 128], BF16, "onesq")
            cx.op("dve", lambda e: e.memset(onesq[:, :], 1.0), [], [onesq])
            s128p = cx.sb(ph, [128, 2 * NSL], F32, "s128p")
            cx.dma("sp", s128p[:, :], d["s128p"][:, :], writes=[s128p], semb=s128p)
            NE = NS * E
            selb = cx.sb(ph, [128, NE], BF16, "selb")
            cx.op("dve", lambda e: e.tensor_copy(out=selb[:, :], in_=selA[:, :, :].rearrange("p j e -> p (j e)")),
                  [selA], [selb])
            mm(cx, pg[0][:, 0:NE], ltri[:, :], selb[:, :], True, True, [ltri, selb], [pg[0]])
            mm(cx, pu[0][:, 0:NE], onesq[:, :], selb[:, :], True, True, [onesq, selb], [pu[0]])
            pre = cx.sb(ph, [128, NS, E], F32, "pre")
            tot = cx.sb(ph, [128, NS, E], F32, "tot")
            cum = cx.sb(ph, [128, NS, E], F32, "cum")
            cx.op("dve", lambda e: e.tensor_copy(out=pre[:, :, :].rearrange("p j e -> p (j e)"), in_=pg[0][:, 0:NE]),
                  [pg[0]], [pre])
            cx.op("dve", lambda e: e.tensor_copy(out=tot[:, :, :].rearrange("p j e -> p (j e)"), in_=pu[0][:, 0:NE]),
                  [pu[0]], [tot])
            cx.op("dve", lambda e: e.memset(cum[:, 0, :], 0.0), [], [cum])
            for j in range(1, NS):
                cx.op("dve", lambda e: e.tensor_tensor(out=cum[:, j, :], in0=cum[:, j - 1, :], in1=tot[:, j - 1, :],
                                                       op=ALU.add), [cum, tot], [cum])
            sc = cx.sb(ph, [128, 6, E], F32, "msc")
            sci = cx.sb(ph, [128, E], I32, "msci")
            cx.op("dve", lambda e: e.tensor_tensor(out=sc[:, 0, :], in0=cum[:, NS - 1, :], in1=tot[:, NS - 1, :],
                                                   op=ALU.add), [cum, tot], [sc])
            cx.op("dve", lambda e: e.tensor_scalar(out=sc[:, 1, :], in0=sc[:, 0, :], scalar1=511.0, scalar2=None,
                                                   op0=ALU.add), [sc], [sc])
            cx.op("dve", lambda e: e.tensor_copy(out=sci[:, :], in_=sc[:, 1, :]), [sc], [sci])
            cx.op("dve", lambda e: e.tensor_scalar(out=sci[:, :], in0=sci[:, :], scalar1=9, scalar2=9,
                                                   op0=ALU.arith_shift_right, op1=ALU.logical_shift_left),
                  [sci], [sci])
            cx.op("dve", lambda e: e.tensor_copy(out=sc[:, 2, :], in_=sci[:, :]), [sci], [sc])
            cx.op("dve", lambda e: e.memset(sc[:, 3, 0:1], 0.0), [], [sc])
            for e_ in range(1, E):
                cx.op("dve", lambda e: e.tensor_tensor(out=sc[:, 3, e_:e_ + 1], in0=sc[:, 3, e_ - 1:e_],
                                                       in1=sc[:, 2, e_ - 1:e_], op=ALU.add), [sc], [sc])
            cx.op("dve", lambda e: e.tensor_tensor(out=sc[:, 4, :], in0=sc[:, 3, :], in1=sc[:, 2, :], op=ALU.add),
                  [sc], [sc])
            cx.op("dve", lambda e: e.tensor_tensor(out=cum[:, :, :], in0=cum[:, :, :], in1=pre[:, :, :], op=ALU.add),
                  [cum, pre], [cum])
            for j in range(NS):
                cx.op("dve", lambda e: e.tensor_tensor(out=cum[:, j, :], in0=cum[:, j, :], in1=sc[:, 3, :],
                                                       op=ALU.add), [cum, sc], [cum])
            dest = cum
            oh2 = pre
            cx.op("dve", lambda e: e.tensor_tensor(out=oh2[:, :, :], in0=selA[:, :, :], in1=oh1A[:, :, :],
                                                   op=ALU.subtract), [selA, oh1A], [oh2])
            prod = tot
            dr = cx.sb(ph, [128, 4, NS], F32, "mdr")
            for qi, (ma, va_) in enumerate(((oh1A, dest), (oh2, dest), (oh1A, rw), (oh2, rw))):
                cx.op("dve", lambda e: e.tensor_tensor(out=prod[:, :, :], in0=ma[:, :, :], in1=va_[:, :, :],
                                                       op=ALU.mult), [ma, va_], [prod])
                cx.op("dve", lambda e: e.reduce_sum(out=dr[:, qi, :], in_=prod[:, :, :], axis=AX.X), [prod], [dr])
            di = cx.sb(ph, [128, 2, NS], I32, "mdi")
            cx.op("dve", lambda e: e.tensor_scalar(out=dr[:, 0:2, :], in0=dr[:, 0:2, :], scalar1=float(NBLK * 512 - 1),
                                                   scalar2=None, op0=ALU.min), [dr], [dr])
            cx.op("dve", lambda e: e.tensor_copy(out=di[:, :, :], in_=dr[:, 0:2, :]), [dr], [di])
            beF = cx.sb(ph, [128, NBLK], F32, "beF")
            for b in range(NBLK):
                cx.op("dve", lambda e: e.tensor_scalar(out=sc[:, 5, :], in0=sc[:, 4, :], scalar1=float(512 * b),
                                                       scalar2=None, op0=ALU.is_le), [sc], [sc])
                cx.op("dve", lambda e: e.reduce_sum(out=beF[:, b:b + 1], in_=sc[:, 5, :], axis=AX.X), [sc], [beF])
            cx.op("dve", lambda e: e.tensor_scalar(out=beF[:, :], in0=beF[:, :], scalar1=float(E - 1),
                                                   scalar2=float(2 * NSL * 128), op0=ALU.min, op1=ALU.mult), [beF], [beF])
            idxF = cx.sb(ph, [128, NBLK * 2 * NSL], F32, "idxF")
            for b in range(NBLK):
                cx.op("dve", lambda e: e.tensor_scalar(out=idxF[:, b * 2 * NSL:(b + 1) * 2 * NSL], in0=s128p[:, :],
                                                       scalar1=beF[:, b:b + 1], scalar2=None, op0=ALU.add),
                      [s128p, beF], [idxF])
            idxI = cx.sb(ph, [128, NBLK * 2 * NSL], I32, "idxI")
            cx.op("dve", lambda e: e.tensor_copy(out=idxI[:, :], in_=idxF[:, :]), [idxF], [idxI])

            xsb = [cx.sb(ph, [128, 4, D], BF16, f"xsb{i}") for i in range(2)]
            hrow = [cx.sb(ph, [128, D], BF16, f"hrow{i}") for i in range(2)]
            for j in range(NS):
                hr = hrow[j % 2]
                cx.dma("sp", hr[:, :], d["H2"][j * 128:(j + 1) * 128, :], writes=[hr], semb=hr)
                for k in range(2):
                    self.idma(d["XS"][:, :], hr[:, :], IOA(ap=di[:, k, j:j + 1], axis=0), None, [hr, di], [], hr)
            cx.barrier(release=False)

            xT = [cx.sb(ph, [128, 8, 512], BF16, f"mxT{i}") for i in range(2)]
            w1s = [cx.sb(ph, [128, 8, 512], BF16, f"mw1{i}") for i in range(3)]
            w3s = [cx.sb(ph, [128, 8, 512], BF16, f"mw3{i}") for i in range(3)]
            w2s = [cx.sb(ph, [128, 4, D], BF16, f"mw2{i}") for i in range(3)]
            sg = [cx.sb(ph, [128, 512], F32, f"msg{i}") for i in range(2)]
            hid = [cx.sb(ph, [128, 4, 512], BF16, f"mhid{i}") for i in range(2)]
            accb = [cx.sb(ph, [128, 4, D], F32, f"macc{i}") for i in range(2)]
            cmb = [[cx.sb(ph, [128, D], F32, f"mcmb{a}{i}") for i in range(2)] for a in range(3)]
            units = [(b, s_) for b in range(NBLK) for s_ in range(NSL)]

            def loadw(n):
                k = n % 3
                for hf in range(2):
                    col = idxI[:, 2 * n + hf:2 * n + hf + 1]
                    self.idma(w1s[k][:, 4 * hf:4 * hf + 4, :].rearrange("p c n -> p (c n)"), d["w1r" + sfx][:, :], None,
                              IOA(ap=col, axis=0), [idxI], [w1s[k]], w1s[k])
                    self.idma(w3s[k][:, 4 * hf:4 * hf + 4, :].rearrange("p c n -> p (c n)"), d["w3r" + sfx][:, :], None,
                              IOA(ap=col, axis=0), [idxI], [w3s[k]], w3s[k])
                    self.idma(w2s[k][:, 2 * hf:2 * hf + 2, :].rearrange("p c n -> p (c n)"), d["w2r" + sfx][:, :], None,
                              IOA(ap=col, axis=0), [idxI], [w2s[k]], w2s[k])

            def prep(b):
                xs = xsb[b % 2]
                cx.dma("sp", xs[:, :, :], d["XS"][b * 512:(b + 1) * 512, :].rearrange("(u p) c -> p u c", p=128),
                       writes=[xs], semb=xs)
                for u in range(4):
                    for c in range(8):
                        cx.op("pe", lambda e: e.transpose(pT[:, c * 128:(c + 1) * 128], xs[:, u, c * 128:(c + 1) * 128],
                                                          ident[:, :]), [xs, ident], [pT])
                    cx.op("act", lambda e: e.copy(out=xT[b % 2][:, :, u * 128:(u + 1) * 128],
                                                  in_=pT[:, :].rearrange("p (c t) -> p c t", c=8)), [pT], [xT[b % 2]])

            def gu(n):
                b, s_ = units[n]
                k = n % 3
                hd = hid[n % 2]
                x_ = xT[b % 2]
                for f in range(4):
                    G_, U_ = pg[f % 2], pu[f % 2]
                    for c in range(8):
                        mm(cx, G_[:, :], w1s[k][:, c, f * 128:(f + 1) * 128], x_[:, c, :], c == 0, c == 7,
                           [w1s[k], x_], [G_])
                    for c in range(8):
                        mm(cx, U_[:, :], w3s[k][:, c, f * 128:(f + 1) * 128], x_[:, c, :], c == 0, c == 7,
                           [w3s[k], x_], [U_])
                    s2 = sg[f % 2]
                    act(cx, s2[:, :], G_[:, :], AF.Silu, [G_], [s2])
                    cx.op("dve", lambda e: e.tensor_tensor(out=hd[:, f, :], in0=s2[:, :], in1=U_[:, :], op=ALU.mult),
                          [s2, U_], [hd])

            def oo(n):
                b, s_ = units[n]
                k = n % 3
                hd = hid[n % 2]
                ac = accb[b % 2]
                for u in range(4):
                    for nh in range(2):
                        O = po[nh]
                        for f in range(4):
                            mm(cx, O[:, :], hd[:, f, u * 128:(u + 1) * 128], w2s[k][:, f, nh * 512:(nh + 1) * 512],
                               f == 0, f == 3, [hd, w2s[k]], [O])
                        sl = slice(nh * 512, (nh + 1) * 512)
                        if s_ == 0:
                            cx.op("dve", lambda e: e.tensor_copy(out=ac[:, u, sl], in_=O[:, :]), [O], [ac])
                        else:
                            cx.op("dve", lambda e: e.tensor_tensor(out=ac[:, u, sl], in0=ac[:, u, sl], in1=O[:, :],
                                                                   op=ALU.add), [ac, O], [ac])
                if s_ == NSL - 1:
                    cx.dma("sp", d["YS"][b * 512:(b + 1) * 512, :].rearrange("(u p) c -> p u c", p=128), ac[:, :, :],
                           reads=[ac], writes=[], semb=ac)

            loadw(0)
            loadw(1)
            loadw(2)
            prep(0)
            gu(0)
            for n in range(len(units)):
                b, s_ = units[n]
                if s_ == 2 and b + 1 < NBLK:
                    prep(b + 1)
                if n + 1 < len(units):
                    gu(n + 1)
                oo(n)
                if n + 3 < len(units):
                    loadw(n + 3)
            cx.barrier(release=False)

            for j in range(NS):
                a_ = T(accb[0].t[:, (j % 2) * 2, :]) if False else None
                a_, b_, x_ = cmb[0][j % 2], cmb[1][j % 2], cmb[2][j % 2]
                cx.dma("sp", x_[:, :], d["X1"][j * 128:(j + 1) * 128, :], writes=[x_], semb=x_)
                self.idma(a_[:, :], d["YS"][:, :], None, IOA(ap=di[:, 0, j:j + 1], axis=0), [di], [a_], a_)
                self.idma(b_[:, :], d["YS"][:, :], None, IOA(ap=di[:, 1, j:j + 1], axis=0), [di], [b_], b_)
                cx.op("dve", lambda e: e.scalar_tensor_tensor(out=x_[:, :], in0=a_[:, :], scalar=dr[:, 2, j:j + 1],
                                                              in1=x_[:, :], op0=ALU.mult, op1=ALU.add),
                      [a_, dr, x_], [x_])
                cx.op("dve", lambda e: e.scalar_tensor_tensor(out=x_[:, :], in0=b_[:, :], scalar=dr[:, 3, j:j + 1],
                                                              in1=x_[:, :], op0=ALU.mult, op1=ALU.add),
                      [b_, dr, x_], [x_])
                cx.dma("sp", d["xout"][j * 128:(j + 1) * 128, :], x_[:, :], reads=[x_], writes=[], semb=x_)
            cx.barrier()

def slot_blocks(p, nslot):
    return [4 * (j // 2) + GSLOT[p][j % 2] for j in range(nslot)]


def gather_own(xb, p):
    S = xb.shape[0]
    blocks = slot_blocks(p, S // 256)
    return np.ascontiguousarray(np.concatenate([xb[g * 128:(g + 1) * 128] for g in blocks], axis=0))


def layer_inputs(inp, L):
    f = lambda a: np.ascontiguousarray(np.asarray(a, dtype=np.float32))
    sfx = f"_{L}"
    m = {}
    m["gmix" + sfx] = f(np.broadcast_to(inp["norm_mix_g"][L][None, :], (128, D)))
    m["w_in" + sfx] = f(inp["w_in"][L])
    gq, gk = inp["diff_q_norm_g"][L], inp["diff_k_norm_g"][L]
    m["gq" + sfx] = f(np.concatenate([gq, gq])[:, None])
    m["gk" + sfx] = f(np.concatenate([gk, gk])[:, None])
    m["gqk_row" + sfx] = f(np.concatenate([gq, gk])[None, :])
    m["lam_row" + sfx] = f(inp["diff_lambda"][L].reshape(1, 256))
    m["gsub" + sfx] = f(np.broadcast_to(inp["diff_subln_g"][L][None, :], (128, 128)))
    m["w_gate" + sfx] = f(inp["w_gate"][L])
    m["b_gate" + sfx] = f(inp["b_gate"][L][None, :])
    m["w_bsb" + sfx] = f(inp["w_branch_sb"][L])
    m["w_bdf" + sfx] = f(inp["w_branch_diff"][L])
    m["w_out" + sfx] = f(inp["w_out"][L])
    m["gffn" + sfx] = f(np.broadcast_to(inp["norm_ffn_g"][L][None, :], (128, D)))
    j = L // 2
    if L % 2 == 0:
        m["w_gu" + sfx] = f(inp["ffn_w_gate_up"][j])
        m["w_dn" + sfx] = f(inp["ffn_w_down"][j])
    else:
        m["w_r" + sfx] = f(inp["moe_w_router"][j])
        w1 = np.asarray(inp["moe_w1"][j], dtype=np.float32)
        w3 = np.asarray(inp["moe_w3"][j], dtype=np.float32)
        w2 = np.asarray(inp["moe_w2"][j], dtype=np.float32)
        rl13 = lambda w: np.ascontiguousarray(
            w.reshape(N_EXP, 2, 4, 128, 7, 512).transpose(0, 4, 1, 3, 2, 5)).reshape(N_EXP * 14 * 128, 2048)
        m["w1r" + sfx] = rl13(w1)
        m["w3r" + sfx] = rl13(w3)
        m["w2r" + sfx] = np.ascontiguousarray(
            w2.reshape(N_EXP, 7, 2, 2, 128, D).transpose(0, 1, 2, 4, 3, 5)).reshape(N_EXP * 14 * 128, 2048)
    return m


_PROG_CACHE = {}


def run_layers(x, inp, layers, debug=False):
    B, S, _ = x.shape
    ncores = 2 * B
    key = (S, tuple(layers), debug)
    if key not in _PROG_CACHE:
        pr = Prog(S, layers, debug, ncores)
        pr.build()
        _PROG_CACHE[key] = pr
    pr = _PROG_CACHE[key]
    in_maps = []
    lw = {}
    for L in layers:
        lw.update(layer_inputs(inp, L))
    for core in range(ncores):
        b, p = core // 2, core % 2
        m = dict(lw)
        m["xfull"] = np.ascontiguousarray(x[b])
        m["xown"] = gather_own(x[b], p)
        hc = host_consts(p, pr.U)
        if not getattr(pr, "has_moe", False):
            hc.pop("ltri"); hc.pop("s128p")
        m.update(hc)
        if len(layers) > 1:
            pm = np.zeros((128, 2), np.float32)
            pm[:, p] = 1.0
            m["pmask"] = pm
        in_maps.append(m)
    res = run_bass_kernel_spmd(pr.nc, in_maps, core_ids=list(range(ncores)))
    out = np.zeros((B, S, D), np.float32)
    dbg = []
    for core in range(ncores):
        b, p = core // 2, core % 2
        r = res.results[core]
        xo = r["xout"]
        for j, g in enumerate(slot_blocks(p, S // 256)):
            out[b, g * 128:(g + 1) * 128] = xo[j * 128:(j + 1) * 128]
        dbg.append(r)
    return out, dbg


def kernel(**inputs):
    inp = {k: np.asarray(v) for k, v in inputs.items()}
    x = np.asarray(inp["x"], dtype=np.float32)
    depth = inp["w_in"].shape[0]
    out, _ = run_layers(x, inp, list(range(depth)))
    return out
```
